# Optimizing a Trainium2 kernel written in Bass

```python
import jax
import jax.numpy as jnp
from jax import lax
import numpy as np

D_MODEL = 1024
BATCH = 2
SEQ = 8192
DEPTH = 1

N_META = 16
EPS = 1e-6

ATT_HEADS = 8
ATT_HEAD_DIM = 64
ATT_WIDTH = ATT_HEADS * ATT_HEAD_DIM
Q_BLOCK = 128

HGRN_HEADS = 8
HGRN_K = 128
HGRN_V = 128
HGRN_KEY_WIDTH = HGRN_HEADS * HGRN_K
HGRN_WIDTH = HGRN_HEADS * HGRN_V
HGRN_CHUNK = 64

N_GROUPS = 4
EXPERTS_PER_GROUP = 8
N_EXPERTS = N_GROUPS * EXPERTS_PER_GROUP
TOP_K = 2
EXPERT_FF = 512
MOE_BLOCK = 128

IN_SIZES = (ATT_WIDTH, ATT_WIDTH, ATT_WIDTH, ATT_HEADS, HGRN_KEY_WIDTH, HGRN_KEY_WIDTH, HGRN_WIDTH, HGRN_WIDTH, D_MODEL, D_MODEL)
IN_WIDTH = sum(IN_SIZES)

kernel_name = 'hybrid_fox_hgrn2_hier_moe_block'


def split_points(sizes):
    pts, acc = [], 0
    for s in sizes[:-1]:
        acc += s
        pts.append(acc)
    return pts


def rms_norm(x, w):
    xf = x.astype(jnp.float32)
    y = xf * lax.rsqrt(jnp.mean(xf * xf, axis=-1, keepdims=True) + EPS)
    return (y * w.astype(jnp.float32)).astype(x.dtype)


def forgetting_attention(q, k, v, log_f):
    B, L, H, Dh = q.shape
    scale = Dh ** -0.5
    c = jnp.cumsum(log_f, axis=1)
    c_k = jnp.transpose(c, (0, 2, 1))

    def attend(q_blk, c_q, q_pos, k_blk, v_blk, c_kb, k_pos):
        s = jnp.einsum('bqhd,bkhd->bhqk', q_blk, k_blk).astype(jnp.float32) * scale
        s = s + jnp.transpose(c_q, (0, 2, 1))[..., :, None] - c_kb[:, :, None, :]
        mask = k_pos[None, :] <= q_pos[:, None]
        s = jnp.where(mask[None, None], s, -jnp.inf)
        p = jax.nn.softmax(s, axis=-1).astype(v_blk.dtype)
        return jnp.einsum('bhqk,bkhd->bqhd', p, v_blk)

    meta_pos = jnp.arange(N_META)
    o_meta = attend(q[:, :N_META], c[:, :N_META], meta_pos, k[:, :N_META], v[:, :N_META], c_k[:, :, :N_META], meta_pos)

    n_blk = (L - N_META) // Q_BLOCK
    k_pos = jnp.arange(L)

    def real_block(i):
        start = N_META + i * Q_BLOCK
        q_blk = lax.dynamic_slice_in_dim(q, start, Q_BLOCK, axis=1)
        c_q = lax.dynamic_slice_in_dim(c, start, Q_BLOCK, axis=1)
        q_pos = start + jnp.arange(Q_BLOCK)
        return attend(q_blk, c_q, q_pos, k, v, c_k, k_pos)

    o_real = lax.map(real_block, jnp.arange(n_blk))
    o_real = jnp.moveaxis(o_real, 0, 1).reshape(B, n_blk * Q_BLOCK, H, Dh)
    return jnp.concatenate([o_meta, o_real], axis=1)


def hgrn2_chunk(state, chunk):
    q, log_f, k, v = chunk
    C = q.shape[1]
    b = jnp.cumsum(log_f, axis=1)
    o_inter = jnp.einsum('bchk,bhkv->bchv', q * jnp.exp(b), state)
    causal = jnp.tril(jnp.ones((C, C), dtype=bool))
    diff = b[:, :, None] - b[:, None, :]
    decay = jnp.exp(jnp.where(causal[None, :, :, None, None], diff, -jnp.inf))
    scores = jnp.sum(q[:, :, None] * k[:, None] * decay, axis=-1)
    o_intra = jnp.einsum('btsh,bshv->bthv', scores, v)
    b_last = b[:, -1]
    k_dec = k * jnp.exp(b_last[:, None] - b)
    new_state = jnp.exp(b_last)[..., None] * state + jnp.einsum('bshk,bshv->bhkv', k_dec, v)
    return new_state, o_inter + o_intra


def hgrn2_recurrence(q, log_f, k, v):
    B, L, H, K = q.shape
    V = v.shape[-1]
    state0 = jnp.zeros((B, H, K, V), jnp.float32)
    state1, o_meta = hgrn2_chunk(state0, (q[:, :N_META], log_f[:, :N_META], k[:, :N_META], v[:, :N_META]))
    n_chunks = (L - N_META) // HGRN_CHUNK

    def to_chunks(a):
        return jnp.moveaxis(a[:, N_META:].reshape(B, n_chunks, HGRN_CHUNK, H, a.shape[-1]), 1, 0)

    _, o_real = lax.scan(hgrn2_chunk, state1, (to_chunks(q), to_chunks(log_f), to_chunks(k), to_chunks(v)))
    o_real = jnp.moveaxis(o_real, 0, 1).reshape(B, L - N_META, H, V)
    return jnp.concatenate([o_meta, o_real], axis=1)


def hierarchical_moe(v2d, w_rg, b_rg, w_re, b_re, w_e_in, w_e_out):
    T, D = v2d.shape
    dtype = v2d.dtype
    vf = v2d.astype(jnp.float32)
    g_logits = vf @ w_rg.astype(jnp.float32) + b_rg.astype(jnp.float32)
    g_prob = jax.nn.softmax(g_logits, axis=-1)
    g_idx = jnp.argmax(g_logits, axis=-1)
    p_g = jnp.take_along_axis(g_prob, g_idx[:, None], axis=1)[:, 0]
    e_logits = (vf @ w_re.astype(jnp.float32) + b_re.astype(jnp.float32)).reshape(T, N_GROUPS, EXPERTS_PER_GROUP)
    e_sel = jnp.take_along_axis(e_logits, g_idx[:, None, None], axis=1)[:, 0]
    top_v, top_i = lax.top_k(e_sel, TOP_K)
    gate = jax.nn.softmax(top_v, axis=-1) * p_g[:, None]
    expert_id = (g_idx[:, None] * EXPERTS_PER_GROUP + top_i).astype(jnp.int32)

    A = T * TOP_K
    eid = expert_id.reshape(-1)
    tok = jnp.repeat(jnp.arange(T, dtype=jnp.int32), TOP_K)
    wt = gate.reshape(-1)
    order = jnp.argsort(eid)
    se, st, sw = eid[order], tok[order], wt[order]
    counts = jnp.bincount(eid, length=N_EXPERTS).astype(jnp.int32)
    starts = jnp.cumsum(counts) - counts
    pcounts = (counts + MOE_BLOCK - 1) // MOE_BLOCK * MOE_BLOCK
    pends = jnp.cumsum(pcounts)
    pstarts = pends - pcounts
    dest = pstarts[se] + jnp.arange(A, dtype=jnp.int32) - starts[se]
    n_blocks = -(-A // MOE_BLOCK) + N_EXPERTS
    rows_tok = jnp.full((n_blocks * MOE_BLOCK,), T, jnp.int32).at[dest].set(st)
    rows_w = jnp.zeros((n_blocks * MOE_BLOCK,), jnp.float32).at[dest].set(sw)
    blk_exp = jnp.clip(jnp.searchsorted(pends, jnp.arange(n_blocks, dtype=jnp.int32) * MOE_BLOCK, side='right'), 0, N_EXPERTS - 1)
    v_pad = jnp.concatenate([v2d, jnp.zeros((1, D), dtype)], axis=0)

    def run(args):
        tok_b, e = args
        xb = v_pad[tok_b]
        hu = xb @ w_e_in[e]
        a, u = jnp.split(hu, 2, axis=-1)
        return (jax.nn.silu(a) * u) @ w_e_out[e]

    ys = lax.map(run, (rows_tok.reshape(n_blocks, MOE_BLOCK), blk_exp)).reshape(-1, D)
    ys = ys * rows_w[:, None].astype(ys.dtype)
    out = jnp.zeros((T + 1, D), ys.dtype).at[rows_tok].add(ys)
    return out[:T].astype(dtype)


def setup_inputs(seed: int = 0) -> dict:
    key = jax.random.key(seed)
    ks = jax.random.split(key, 18)
    n = jax.random.normal
    f32 = jnp.float32
    return {
        'x': n(ks[0], (BATCH, SEQ, D_MODEL), f32),
        'meta_tokens': n(ks[1], (N_META, D_MODEL), f32),
        'norm_mix_w': 1.0 + 0.02 * n(ks[2], (DEPTH, D_MODEL), f32),
        'w_in': n(ks[3], (DEPTH, D_MODEL, IN_WIDTH), f32) * D_MODEL ** -0.5,
        'attn_forget_b': 3.0 + 0.5 * n(ks[4], (DEPTH, ATT_HEADS), f32),
        'hgrn_lb_logits': 0.1 * n(ks[5], (DEPTH + 1, HGRN_KEY_WIDTH), f32),
        'hgrn_norm_w': 1.0 + 0.02 * n(ks[6], (DEPTH, HGRN_V), f32),
        'w_attn_out': n(ks[7], (DEPTH, ATT_WIDTH, D_MODEL), f32) * ATT_WIDTH ** -0.5,
        'w_hgrn_out': n(ks[8], (DEPTH, HGRN_WIDTH, D_MODEL), f32) * HGRN_WIDTH ** -0.5,
        'w_o': n(ks[9], (DEPTH, D_MODEL, D_MODEL), f32) * D_MODEL ** -0.5,
        'norm_ffn_w': 1.0 + 0.02 * n(ks[10], (DEPTH, D_MODEL), f32),
        'w_router_group': n(ks[11], (DEPTH, D_MODEL, N_GROUPS), f32) * D_MODEL ** -0.5,
        'b_router_group': 0.01 * n(ks[12], (DEPTH, N_GROUPS), f32),
        'w_router_expert': n(ks[13], (DEPTH, D_MODEL, N_EXPERTS), f32) * D_MODEL ** -0.5,
        'b_router_expert': 0.01 * n(ks[14], (DEPTH, N_EXPERTS), f32),
        'w_expert_in': n(ks[15], (DEPTH, N_EXPERTS, D_MODEL, 2 * EXPERT_FF), f32) * D_MODEL ** -0.5,
        'w_expert_out': n(ks[16], (DEPTH, N_EXPERTS, EXPERT_FF, D_MODEL), f32) * EXPERT_FF ** -0.5,
        'norm_final_w': 1.0 + 0.02 * n(ks[17], (D_MODEL,), f32),
    }


def reference(x, meta_tokens, norm_mix_w, w_in, attn_forget_b, hgrn_lb_logits, hgrn_norm_w, w_attn_out, w_hgrn_out, w_o, norm_ffn_w, w_router_group, b_router_group, w_router_expert, b_router_expert, w_expert_in, w_expert_out, norm_final_w):
    B, S, D = x.shape
    L = S + N_META
    meta = jnp.broadcast_to(meta_tokens.astype(x.dtype)[None], (B, N_META, D))
    h = jnp.concatenate([meta, x], axis=1)
    lb_all = jnp.cumsum(jax.nn.softmax(hgrn_lb_logits.astype(jnp.float32), axis=0), axis=0)
    for layer in range(DEPTH):
        u = rms_norm(h, norm_mix_w[layer])
        proj = u @ w_in[layer]
        q_a, k_a, v_a, f_a, q_h, f_h, i_h, g_h, gate_a, gate_h = jnp.split(proj, split_points(IN_SIZES), axis=-1)

        log_f_a = jax.nn.log_sigmoid(f_a.astype(jnp.float32) + attn_forget_b[layer].astype(jnp.float32))
        att_shape = (B, L, ATT_HEADS, ATT_HEAD_DIM)
        o_a = forgetting_attention(q_a.reshape(att_shape), k_a.reshape(att_shape), v_a.reshape(att_shape), log_f_a)
        y_a = o_a.reshape(B, L, ATT_WIDTH) @ w_attn_out[layer]

        lb = lb_all[layer]
        f_val = lb + (1.0 - lb) * jax.nn.sigmoid(f_h.astype(jnp.float32))
        key_shape = (B, L, HGRN_HEADS, HGRN_K)
        log_f_h = jnp.log(f_val).reshape(key_shape)
        k_h = (1.0 - f_val).reshape(key_shape)
        q_hr = q_h.astype(jnp.float32).reshape(key_shape) * HGRN_K ** -0.5
        v_h = i_h.astype(jnp.float32).reshape(B, L, HGRN_HEADS, HGRN_V)
        o_h = hgrn2_recurrence(q_hr, log_f_h, k_h, v_h)
        o_h = rms_norm(o_h, hgrn_norm_w[layer]) * jax.nn.sigmoid(g_h.astype(jnp.float32)).reshape(B, L, HGRN_HEADS, HGRN_V)
        y_h = o_h.reshape(B, L, HGRN_WIDTH).astype(x.dtype) @ w_hgrn_out[layer]

        mixed = jax.nn.sigmoid(gate_a) * y_a + jax.nn.sigmoid(gate_h) * y_h
        h = h + mixed @ w_o[layer]

        v_in = rms_norm(h, norm_ffn_w[layer]).reshape(B * L, D)
        moe_out = hierarchical_moe(v_in, w_router_group[layer], b_router_group[layer], w_router_expert[layer], b_router_expert[layer], w_expert_in[layer], w_expert_out[layer])
        h = h + moe_out.reshape(B, L, D)
    return rms_norm(h, norm_final_w)[:, N_META:]
```

```python
import contextlib
import numpy as np
from concourse.bass_utils import run_bass_kernel_spmd
import concourse.bass as bass
import concourse.mybir as mybir

F32 = mybir.dt.float32
BF16 = mybir.dt.bfloat16
I32 = mybir.dt.int32
AF = mybir.ActivationFunctionType
ALU = mybir.AluOpType
AX = mybir.AxisListType

ENGS = ["pe", "act", "dve", "pool", "sp"]
NDMASEM = 6


class _Op:
    __slots__ = ("eng", "fn", "waits", "seq", "signal", "dma_slot", "dma_n", "is_dma")


class Prog:
    def __init__(self, nc, same_eng_sync=True):
        self.nc = nc
        self.same = same_eng_sync
        self.ops = {e: [] for e in ENGS}
        self.last_w = {}
        self.readers = {}
        self.known = {e: {} for e in ENGS}
        self.dma_rot = {e: 0 for e in ENGS}
        self.dma_last = {}
        self.dma_cnt = {}

    @staticmethod
    def _key(op):
        return ("dma", op.eng, op.dma_slot) if op.is_dma else op.eng

    def _need(self, op, dep, waits):
        if dep is None:
            return
        k = self._key(dep)
        if (not dep.is_dma) and dep.eng == op.eng and (op.eng == "pe" or not self.same):
            return
        v = dep.dma_n if dep.is_dma else dep.seq
        if self.known[op.eng].get(k, 0) >= v:
            return
        cur = waits.get(k)
        if cur is None or (cur.dma_n if cur.is_dma else cur.seq) < v:
            waits[k] = dep

    @staticmethod
    def rk(r):
        if isinstance(r, (str, int)):
            return r
        if isinstance(r, tuple):
            return tuple(Prog.rk(x) for x in r)
        return id(r)

    def op(self, eng, fn, reads=(), writes=(), dma=False):
        reads = [self.rk(r) for r in reads]
        writes = [self.rk(r) for r in writes]
        o = _Op()
        o.eng = eng
        o.fn = fn
        o.is_dma = dma
        o.signal = False
        o.seq = len(self.ops[eng]) + 1
        o.dma_slot = None
        o.dma_n = 0
        waits = {}
        if dma:
            s = self.dma_rot[eng]
            self.dma_rot[eng] = (s + 1) % NDMASEM
            o.dma_slot = s
            n = self.dma_cnt.get((eng, s), 0) + 1
            self.dma_cnt[(eng, s)] = n
            o.dma_n = n
            prev = self.dma_last.get((eng, s))
            self._need(o, prev, waits)
            self.dma_last[(eng, s)] = o
        for r in reads:
            self._need(o, self.last_w.get(r), waits)
        for r in writes:
            self._need(o, self.last_w.get(r), waits)
            for d in self.readers.get(r, {}).values():
                if d is not o:
                    self._need(o, d, waits)
        for k, d in waits.items():
            d.signal = True
            self.known[eng][k] = d.dma_n if d.is_dma else d.seq
        o.waits = list(waits.values())
        for r in reads:
            self.readers.setdefault(r, {})[self._key(o)] = o
        for r in writes:
            self.last_w[r] = o
            self.readers[r] = {}
        self.ops[eng].append(o)
        return o

    def emit(self, final_waits_eng="sp"):
        nc = self.nc
        for e in ENGS:
            for o in self.ops[e]:
                if o.is_dma:
                    o.signal = True
        tails = []
        for e in ENGS:
            last = None
            for o in self.ops[e]:
                if not o.is_dma:
                    last = o
            if last is not None:
                last.signal = True
                tails.append(last)
        for k, o in self.dma_last.items():
            tails.append(o)
        val = {}
        for e in ENGS:
            c = 0
            for o in self.ops[e]:
                if o.is_dma:
                    val[id(o)] = 16 * o.dma_n
                elif o.signal:
                    c += 1
                    val[id(o)] = c
        import contextlib
        with contextlib.ExitStack() as st:
            sem = {}
            for e in ENGS:
                sem[e] = st.enter_context(nc.semaphore("s_" + e))
                for s in range(NDMASEM):
                    if (e, s) in self.dma_cnt:
                        sem[("dma", e, s)] = st.enter_context(nc.semaphore("d_%s%d" % (e, s)))
            block = st.enter_context(nc.Block())
            prog = self

            def run(e, h):
                for o in prog.ops[e]:
                    for d in o.waits:
                        h.wait_ge(sem[prog._key(d)], val[id(d)])
                    ins = o.fn(h)
                    if o.is_dma:
                        ins.then_inc(sem[prog._key(o)], 16)
                    elif o.signal:
                        ins.then_inc(sem[e], 1)
                if e == final_waits_eng:
                    for d in tails:
                        h.wait_ge(sem[prog._key(d)], val[id(d)])

            @block.tensor
            def _(h):
                run("pe", h)

            @block.scalar
            def _(h):
                run("act", h)

            @block.vector
            def _(h):
                run("dve", h)

            @block.gpsimd
            def _(h):
                run("pool", h)

            @block.sync
            def _(h):
                run("sp", h)


NB = 17
NT = 68
NPOS = 8704
QA, KA, VA, FA, QH, FH, IH, GH = 0, 128, 256, 384, 386, 642, 898, 1154
W1C = 1410


class Ops:
    def __init__(self, P):
        self.P = P

    def MM(self, out, lhsT, rhs, st, sp, r, w):
        self.P.op("pe", lambda h: h.matmul(out, lhsT=lhsT, rhs=rhs, start=st, stop=sp), r, w)

    def TR(self, out, in_, ident, r, w):
        self.P.op("pe", lambda h: h.transpose(out, in_, ident), r, w)

    def ACT(self, out, in_, func, r, w, bias=None, scale=None):
        kw = {}
        if bias is not None:
            kw["bias"] = bias
        if scale is not None:
            kw["scale"] = scale
        self.P.op("act", lambda h: h.activation(out=out, in_=in_, func=func, **kw), r, w)

    def ACOPY(self, out, in_, r, w):
        self.P.op("act", lambda h: h.copy(out=out, in_=in_), r, w)

    def CP(self, eng, out, in_, r, w):
        if eng == "act":
            return self.ACOPY(out, in_, r, w)
        self.P.op(eng, lambda h: h.tensor_copy(out=out, in_=in_), r, w)

    def TS(self, eng, out, in0, s1, s2, op0, op1, r, w):
        if op1 is None:
            self.P.op(eng, lambda h: h.tensor_scalar(out=out, in0=in0, scalar1=s1, scalar2=None, op0=op0), r, w)
        else:
            self.P.op(eng, lambda h: h.tensor_scalar(out=out, in0=in0, scalar1=s1, scalar2=s2, op0=op0, op1=op1), r, w)

    def TT(self, eng, out, in0, in1, op, r, w):
        self.P.op(eng, lambda h: h.tensor_tensor(out=out, in0=in0, in1=in1, op=op), r, w)

    def STT(self, out, in0, scalar, in1, op0, op1, r, w):
        self.P.op("dve", lambda h: h.scalar_tensor_tensor(out=out, in0=in0, scalar=scalar, in1=in1, op0=op0, op1=op1), r, w)

    def RECIP(self, out, in_, r, w):
        self.P.op("dve", lambda h: h.reciprocal(out=out, in_=in_), r, w)

    def MEMSET(self, eng, ap, val, w):
        self.P.op(eng, lambda h: h.memset(ap, val), [], w)

    def DMA(self, q, out, in_, r, w):
        self.P.op(q, lambda h: h.dma_start(out=out, in_=in_), r, w, dma=True)


def build1(nblk=NB, stage=99):
    NPOS_ = nblk * 512
    NT_ = nblk * 4
    NR = max((nblk - 1) * 512, 512)
    nc = bass.Bass("TRN2", target_bir_lowering=False)
    xs = nc.dram_tensor("xs", [NPOS_, 1024], F32, kind="ExternalInput").ap()
    w1 = nc.dram_tensor("w1", [1024, W1C], F32, kind="ExternalInput").ap()
    nwb = nc.dram_tensor("nwb", [128, 1024], F32, kind="ExternalInput").ap()
    afb = nc.dram_tensor("afb", [128, 2], F32, kind="ExternalInput").ap()
    lbl = nc.dram_tensor("lbl", [128, 4], F32, kind="ExternalInput").ap()
    hnw = nc.dram_tensor("hnw", [128, 1], F32, kind="ExternalInput").ap()
    oa = nc.dram_tensor("oa", [128, NR], BF16, kind="ExternalOutput").ap()
    oh = nc.dram_tensor("oh", [256, NR], BF16, kind="ExternalOutput").ap()
    with contextlib.ExitStack() as st:
        def sb(name, shape, dt):
            return st.enter_context(nc.sbuf_tensor(name, shape, dt))

        def ps(name, shape, dt):
            return st.enter_context(nc.psum_tensor(name, shape, dt))

        P = Prog(nc)
        O = Ops(P)
        W = sb("W", [128, 8, W1C], BF16)
        nw = sb("nw", [128, 1024], F32)
        afb_sb = sb("afb_sb", [128, 2], F32)
        lbl_sb = sb("lbl_sb", [128, 4], F32)
        hnw_sb = sb("hnw_sb", [128, 1], F32)
        identb = sb("identb", [128, 128], BF16)
        identf = sb("identf", [128, 128], F32)
        ones_b = sb("ones_b", [128, 128], BF16)
        ones_f = sb("ones_f", [128, 128], F32)
        tri_f = sb("tri_f", [128, 128], F32)
        sel0 = sb("sel0", [128, 128], F32)
        rmask = sb("rmask", [128, 512], F32)
        xt = [sb("xt%d" % i, [128, 1024], F32) for i in range(2)]
        junk = sb("junk", [128, 1024], BF16)
        ubf = [sb("ubf%d" % i, [128, 1024], BF16) for i in range(2)]
        ss = [sb("ss%d" % i, [128, 1], F32) for i in range(2)]
        lnv = [sb("lnv%d" % i, [128, 1], F32) for i in range(2)]
        rstd = [sb("rstd%d" % i, [128, 1], F32) for i in range(2)]
        nfk = sb("nfk", [128, NT_], F32)
        uT = [sb("uT%d" % i, [128, 8, 512], BF16) for i in range(2)]
        kT = sb("kT", [128, NPOS_], BF16)
        V = sb("V", [128, NT_, 2, 128], BF16)
        cnegK = sb("cnegK", [128, NT_, 2], F32)
        cumL = sb("cumL", [128, 2], F32)
        fat = [sb("fat%d" % i, [128, 2], F32) for i in range(2)]
        Lt = [sb("Lt%d" % i, [128, 2], F32) for i in range(2)]
        qT = [sb("qT%d" % i, [128, 512], BF16) for i in range(2)]
        vh = [sb("vh%d" % i, [128, 4, 256], BF16) for i in range(2)]
        qraw = [sb("qraw%d" % i, [128, 512], BF16) for i in range(2)]
        tmp = [[sb("tmp%d_%d" % (h, i), [128, 512], F32) for i in range(4)] for h in range(2)]
        QdT = [sb("QdT%d" % i, [128, 512], BF16) for i in range(2)]
        KdT = [sb("KdT%d" % i, [128, 512], BF16) for i in range(2)]
        KddT = [sb("KddT%d" % i, [128, 512], BF16) for i in range(2)]
        bm = [sb("bm%d" % i, [128, 4], F32) for i in range(2)]
        nbm = [sb("nbm%d" % i, [128, 4], F32) for i in range(2)]
        bl = [sb("bl%d" % i, [128, 4], F32) for i in range(2)]
        ebl = [sb("ebl%d" % i, [128, 4], F32) for i in range(2)]
        ebm = [sb("ebm%d" % i, [128, 4], F32) for i in range(2)]
        eblm = [sb("eblm%d" % i, [128, 4], F32) for i in range(2)]
        gsT = [sb("gsT%d" % i, [128, 512], BF16) for i in range(2)]
        S = [sb("S%d" % i, [128, 128], F32) for i in range(2)]
        Sb = [sb("Sb%d" % i, [128, 128], BF16) for i in range(2)]
        KddC = [sb("KddC%d" % i, [128, 128], BF16) for i in range(2)]
        AT = [sb("AT%d" % i, [128, 128], BF16) for i in range(2)]
        oraw = [sb("oraw%d" % i, [128, 512], F32) for i in range(2)]
        sq = [sb("sq%d" % i, [128, 512], BF16) for i in range(2)]
        ohst = [sb("ohst%d" % i, [128, 512], BF16) for i in range(2)]
        lb = sb("lb", [128, 2], F32)
        lbt = sb("lbt", [128, 2], F32)
        bias_blk = sb("bias_blk", [128, NT_, 2], F32)
        ref_sb = sb("ref_sb", [128, 2], F32)
        PT = [sb("PT%d" % i, [128, 512], BF16) for i in range(2)]
        rd = sb("rd", [128, 512], F32)
        bc_sb = sb("bc_sb", [64, 512], F32)
        oast = [sb("oast%d" % i, [64, 512], BF16) for i in range(2)]
        psT = ps("psT", [128, 1024], BF16)
        psI = [ps("psI%d" % i, [128, 512], F32) for i in range(2)]
        psHa = ps("psHa", [128, 512], F32)
        psHb = ps("psHb", [128, 512], F32)
        psS2 = [ps("psS2%d" % i, [128, 512], F32) for i in range(2)]
        psO = ps("psO", [128, 512], F32)
        rot = {"i": 0, "s": 0}

        def nextI():
            rot["i"] ^= 1
            return psI[rot["i"]]

        def nextS():
            rot["s"] ^= 1
            return rot["s"]

        PSTK = [("psT", i) for i in range(8)]
        w1v = w1.rearrange("(k p) c -> p k c", p=128)
        for a, b in [(0, 386), (386, 898), (898, W1C)]:
            O.DMA("pool", W[:, :, a:b], w1v[:, :, a:b], [], [W])
        O.DMA("sp", nw[:], nwb, [], [nw])
        O.DMA("sp", afb_sb[:], afb, [], [afb_sb])
        O.DMA("sp", lbl_sb[:], lbl, [], [lbl_sb])
        O.DMA("sp", hnw_sb[:], hnw, [], [hnw_sb])
        O.MEMSET("pool", ones_f[:], 1.0, [ones_f])
        O.MEMSET("pool", ones_b[:], 1.0, [ones_b])
        P.op("pool", lambda h: h.affine_select(out=identb[:], in_=ones_b[:], pattern=[[1, 128]], compare_op=ALU.is_equal, fill=0.0, base=0, channel_multiplier=-1), [ones_b], [identb])
        P.op("pool", lambda h: h.affine_select(out=identf[:], in_=ones_f[:], pattern=[[1, 128]], compare_op=ALU.is_equal, fill=0.0, base=0, channel_multiplier=-1), [ones_f], [identf])
        P.op("pool", lambda h: h.affine_select(out=tri_f[:], in_=ones_f[:], pattern=[[1, 128]], compare_op=ALU.is_ge, fill=0.0, base=0, channel_multiplier=-1), [ones_f], [tri_f])
        P.op("pool", lambda h: h.affine_select(out=sel0[:], in_=ones_f[:], pattern=[[0, 128]], compare_op=ALU.is_equal, fill=0.0, base=0, channel_multiplier=1), [ones_f], [sel0])
        O.MEMSET("pool", rmask[:], 1.0, [rmask])
        O.MEMSET("pool", rmask[:].rearrange("p (c s) -> p c s", s=128)[:, :, 0:1], 0.0, [rmask])
        O.MEMSET("pool", V[:], 1.0, [V])
        O.MEMSET("pool", cumL[:], 0.0, [cumL])
        for h in range(2):
            O.MEMSET("pool", S[h][:], 0.0, [S[h]])
        O.TT("dve", lbt[:], lbl_sb[:, 2:4], lbl_sb[:, 0:2], ALU.subtract, [lbl_sb], [lbt])
        O.ACT(lbt[:], lbt[:], AF.Exp, [lbt], [lbt])
        O.TS("dve", lbt[:], lbt[:], 1.0, None, ALU.add, None, [lbt], [lbt])
        O.RECIP(lb[:], lbt[:], [lbt], [lb])

        for blk in range(nblk):
            pos0 = blk * 512
            ub = blk % 2
            UT = uT[ub]
            for t in range(4):
                T = 4 * blk + t
                xb = T % 2
                O.DMA("sp", xt[xb][:], xs[T * 128:(T + 1) * 128, :], [], [xt[xb]])
                P.op("dve", lambda h, xb=xb: h.scalar_tensor_tensor(out=junk[:], in0=xt[xb][:], scalar=1.0, in1=xt[xb][:], op0=ALU.mult, op1=ALU.mult, accum_out=ss[xb][:]), [xt[xb]], [junk, ss[xb]])
                O.ACT(lnv[xb][:], ss[xb][:], AF.Ln, [ss[xb]], [lnv[xb]], bias=1e-6, scale=1.0 / 1024)
                O.ACT(rstd[xb][:], lnv[xb][:], AF.Exp, [lnv[xb]], [rstd[xb]], scale=-0.5)
                O.STT(ubf[xb][:], xt[xb][:], rstd[xb][:], nw[:], ALU.mult, ALU.mult, [xt[xb], rstd[xb], nw], [ubf[xb]])
                O.TS("dve", nfk[:, T:T + 1], ss[xb][:], 1e-12, -30000.0, ALU.is_lt, ALU.mult, [ss[xb]], [("nfk", T)])
                for c in range(8):
                    O.TR(psT[:, c * 128:(c + 1) * 128], ubf[xb][:, c * 128:(c + 1) * 128], identb[:], [ubf[xb], identb], PSTK)
                O.ACOPY(UT[:, :, t * 128:(t + 1) * 128], psT[:].rearrange("p (c s) -> p c s", s=128), PSTK, [UT])
            if stage < 2:
                continue

            def fm_chunk(col):
                p_ = nextI()
                for k in range(8):
                    O.MM(p_[:, :], W[:, k, col:col + 128], UT[:, k, :], k == 0, k == 7, [W, UT], [p_])
                return p_

            p_ = fm_chunk(QA)
            O.ACOPY(qT[ub][:], p_[:], [p_], [qT[ub]])
            p_ = fm_chunk(KA)
            O.ACOPY(kT[:, pos0:pos0 + 512], p_[:], [p_], [("kT", blk)])
            for t in range(4):
                T = 4 * blk + t
                p_ = nextI()
                for k in range(8):
                    O.MM(p_[:, 0:130], UT[:, k, t * 128:(t + 1) * 128], W[:, k, VA:VA + 130], k == 0, k == 7, [W, UT], [p_])
                O.CP("dve", V[:, T, :, 0:64], p_[:, 0:128].rearrange("p (h d) -> p h d", d=64), [p_], [("V", T)])
                fb = T % 2
                O.TT("dve", fat[fb][:], p_[:, 128:130], afb_sb[:], ALU.add, [p_, afb_sb], [fat[fb]])
                O.ACT(fat[fb][:], fat[fb][:], AF.Exp, [fat[fb]], [fat[fb]], scale=-1.0)
                O.ACT(Lt[fb][:], fat[fb][:], AF.Ln, [fat[fb]], [Lt[fb]], bias=1.0)
                pc = nextI()
                O.MM(pc[:, 0:2], tri_f[:], Lt[fb][:], True, False, [tri_f, Lt[fb]], [pc])
                O.MM(pc[:, 0:2], ones_f[:], cumL[:], False, True, [ones_f, cumL], [pc])
                O.TS("dve", cnegK[:, T, :], pc[:, 0:2], nfk[:, T:T + 1], None, ALU.add, None, [pc, ("nfk", T)], [("cnegK", T)])
                O.TT("dve", cumL[:], cumL[:], Lt[fb][:], ALU.add, [cumL, Lt[fb]], [cumL])
            if stage < 4:
                continue
            for t in range(4):
                p_ = nextI()
                for k in range(8):
                    O.MM(p_[:, 0:256], UT[:, k, t * 128:(t + 1) * 128], W[:, k, IH:IH + 256], k == 0, k == 7, [W, UT], [p_])
                O.CP("act" if t % 2 else "dve", vh[ub][:, t, :], p_[:, 0:256], [p_], [vh[ub]])
            for h in range(2):
                p_ = fm_chunk(QH + h * 128)
                O.CP("dve", qraw[h][:], p_[:], [p_], [qraw[h]])
            for h in range(2):
                t0, t1, t2, t3 = tmp[h]
                p_ = fm_chunk(FH + h * 128)
                O.ACT(t0[:], p_[:], AF.Exp, [p_], [t0], scale=-1.0)
                O.ACT(t1[:], t0[:], AF.Ln, [t0, lb], [t1], bias=1.0, scale=lb[:, h:h + 1])
                O.ACT(t2[:], t0[:], AF.Ln, [t0], [t2], bias=1.0)
                O.TT("dve", t1[:], t1[:], t2[:], ALU.subtract, [t1, t2], [t1])
                O.ACT(t2[:], t1[:], AF.Exp, [t1], [t2])
                O.TS("dve", t2[:], t2[:], -1.0, 1.0, ALU.mult, ALU.add, [t2], [t2])
                P.op("dve", lambda hh, t1=t1, t3=t3: hh.tensor_tensor_scan(out=t3[:], data0=rmask[:], data1=t1[:], initial=0.0, op0=ALU.mult, op1=ALU.add), [rmask, t1], [t3])
                t3v = t3[:].rearrange("p (c s) -> p c s", s=128)
                O.CP("dve", bm[h][:].rearrange("p (c o) -> p c o", o=1), t3v[:, :, 63:64], [t3], [bm[h]])
                O.CP("dve", bl[h][:].rearrange("p (c o) -> p c o", o=1), t3v[:, :, 127:128], [t3], [bl[h]])
                O.TS("dve", nbm[h][:], bm[h][:], -1.0, None, ALU.mult, None, [bm[h]], [nbm[h]])
                O.ACT(ebl[h][:], bl[h][:], AF.Exp, [bl[h]], [ebl[h]])
                O.ACT(ebm[h][:], bm[h][:], AF.Exp, [bm[h]], [ebm[h]])
                O.TT("dve", eblm[h][:], bl[h][:], bm[h][:], ALU.subtract, [bl[h], bm[h]], [eblm[h]])
                O.ACT(eblm[h][:], eblm[h][:], AF.Exp, [eblm[h]], [eblm[h]])
                for c in range(4):
                    cs = slice(c * 128, (c + 1) * 128)
                    O.ACT(t0[:, cs], t3[:, cs], AF.Exp, [t3, nbm[h]], [t0], bias=nbm[h][:, c:c + 1])
                    O.ACT(t1[:, cs], t3[:, cs], AF.Exp, [t3, bm[h]], [t1], bias=bm[h][:, c:c + 1], scale=-1.0)
                O.STT(QdT[h][:], qraw[h][:], float(128 ** -0.5), t0[:], ALU.mult, ALU.mult, [qraw[h], t0], [QdT[h]])
                O.TT("dve", KdT[h][:], t2[:], t1[:], ALU.mult, [t2, t1], [KdT[h]])
                for c in range(4):
                    cs = slice(c * 128, (c + 1) * 128)
                    O.TS("pool", KddT[h][:, cs], KdT[h][:, cs], eblm[h][:, c:c + 1], None, ALU.mult, None, [KdT[h], eblm[h]], [KddT[h]])
            for h in range(2):
                t0 = tmp[h][0]
                p_ = fm_chunk(GH + h * 128)
                O.ACT(t0[:], p_[:], AF.Exp, [p_], [t0], scale=-1.0)
                O.TS("dve", t0[:], t0[:], 1.0, None, ALU.add, None, [t0], [t0])
                O.RECIP(t0[:], t0[:], [t0], [t0])
                O.TS("dve", gsT[h][:], t0[:], hnw_sb[:, 0:1], None, ALU.mult, None, [t0, hnw_sb], [gsT[h]])
            if stage < 5:
                continue
            for c in range(4):
                cs = slice(c * 128, (c + 1) * 128)
                for h in range(2):
                    A_ = psHa[:, h * 128:(h + 1) * 128]
                    Sg = psHa[:, 256 + h * 128:256 + (h + 1) * 128]
                    O_ = psHb[:, h * 128:(h + 1) * 128]
                    pTr = psT[:, h * 128:(h + 1) * 128]
                    Vc = vh[ub][:, c, h * 128:(h + 1) * 128]
                    O.TR(pTr, KddT[h][:, cs], identb[:], [KddT[h], identb], [("psT", h)])
                    O.ACOPY(KddC[h][:], pTr, [("psT", h)], [KddC[h]])
                    O.TS("dve", Sb[h][:], S[h][:], ebm[h][:, c:c + 1], None, ALU.mult, None, [S[h], ebm[h]], [Sb[h]])
                    O.MM(A_, KdT[h][:, cs], QdT[h][:, cs], True, True, [KdT[h], QdT[h]], [("psHa", "A", h)])
                    O.TT("dve", AT[h][:], A_, tri_f[:], ALU.mult, [("psHa", "A", h), tri_f], [AT[h]])
                    O.MM(O_, Vc, AT[h][:], True, False, [vh[ub], AT[h]], [("psHb", h)])
                    O.MM(O_, Sb[h][:], QdT[h][:, cs], False, True, [Sb[h], QdT[h]], [("psHb", h)])
                    O.ACOPY(oraw[h][:, cs], O_, [("psHb", h)], [oraw[h]])
                    O.MM(Sg, KddC[h][:], Vc, True, True, [KddC[h], vh[ub]], [("psHa", "S", h)])
                    O.STT(S[h][:], S[h][:], ebl[h][:, c:c + 1], Sg, ALU.mult, ALU.add, [S[h], ebl[h], ("psHa", "S", h)], [S[h]])
            if blk == 0 or stage < 6:
                continue
            for h in range(2):
                t0, t1 = tmp[h][0], tmp[h][1]
                O.TT("dve", sq[h][:], oraw[h][:], oraw[h][:], ALU.mult, [oraw[h]], [sq[h]])
                p_ = nextI()
                O.MM(p_[:, :], ones_b[:], sq[h][:], True, True, [ones_b, sq[h]], [p_])
                O.ACT(t0[:], p_[:], AF.Ln, [p_], [t0], bias=1e-6, scale=1.0 / 128)
                O.ACT(t0[:], t0[:], AF.Exp, [t0], [t0], scale=-0.5)
                O.TT("dve", t1[:], oraw[h][:], t0[:], ALU.mult, [oraw[h], t0], [t1])
                O.TT("dve", ohst[h][:], t1[:], gsT[h][:], ALU.mult, [t1, gsT[h]], [ohst[h]])
                O.DMA("sp", oh[h * 128:(h + 1) * 128, pos0 - 512:pos0], ohst[h][:], [ohst[h]], [])
            if stage < 7:
                continue
            ntile = 4 * blk + 4
            pr = nextI()
            O.MM(pr[:, 0:2], sel0[:], cnegK[:, 4 * blk + 2, :], True, True, [sel0, ("cnegK", 4 * blk + 2)], [pr])
            O.ACOPY(ref_sb[:], pr[:, 0:2], [pr], [ref_sb])
            ck = [("cnegK", T) for T in range(3, ntile)]
            for h in range(2):
                O.TS("dve", bias_blk[:, 3:ntile, h], cnegK[:, 3:ntile, h], ref_sb[:, h:h + 1], None, ALU.subtract, None, ck + [ref_sb], [("bias", h)])
            for h in range(2):
                hp = slice(h * 64, (h + 1) * 64)
                po = psO
                for kt in range(3, ntile):
                    r = kt - 4 * blk
                    c0 = 128 * r if r > 0 else 0
                    sr = nextS()
                    O.MM(psS2[sr][:, c0:512], kT[hp, kt * 128:(kt + 1) * 128], qT[ub][hp, c0:512], True, True, [("kT", kt // 4), qT[ub]], [psS2[sr]])
                    O.ACT(PT[sr][:, c0:512], psS2[sr][:, c0:512], AF.Exp, [psS2[sr], ("bias", h)], [PT[sr]], bias=bias_blk[:, kt, h:h + 1], scale=0.125)
                    if r >= 0:
                        O.TT("pool", PT[sr][:, c0:c0 + 128], PT[sr][:, c0:c0 + 128], tri_f[:], ALU.mult, [PT[sr], tri_f], [PT[sr]])
                    O.MM(po[:, c0:512], V[:, kt, h, :], PT[sr][:, c0:512], kt == 3, kt == ntile - 1, [("V", kt), PT[sr]], [po])
                O.RECIP(rd[64:128, :], po[64:128, :], [po], [rd])
                sr = nextS()
                O.MM(psS2[sr][0:64, :], identf[64:128, 64:128], rd[64:128, :], True, True, [identf, rd], [psS2[sr]])
                O.ACOPY(bc_sb[:], psS2[sr][0:64, :], [psS2[sr]], [bc_sb])
                O.TT("dve", oast[h][:], po[0:64, :], bc_sb[:], ALU.mult, [po, bc_sb], [oast[h]])
                O.DMA("sp", oa[h * 64:(h + 1) * 64, pos0 - 512:pos0], oast[h][:], [oast[h]], [])
        P.emit()
    return nc


def build2(ntok=2048, nexp=32):
    NTL = ntok // 128
    NTB = ntok // 512
    nc = bass.Bass("TRN2", target_bir_lowering=False)
    x2 = nc.dram_tensor("x2", [ntok, 1024], F32, kind="ExternalInput").ap()
    oaT = nc.dram_tensor("oaT", [512, ntok], BF16, kind="ExternalInput").ap()
    ohT = nc.dram_tensor("ohT", [1024, ntok], BF16, kind="ExternalInput").ap()
    wg = nc.dram_tensor("wg", [1024, 2048], F32, kind="ExternalInput").ap()
    nwb = nc.dram_tensor("nwb", [3, 128, 1024], F32, kind="ExternalInput").ap()
    w_ao = nc.dram_tensor("w_ao", [512, 1024], F32, kind="ExternalInput").ap()
    w_ho = nc.dram_tensor("w_ho", [1024, 1024], F32, kind="ExternalInput").ap()
    w_o = nc.dram_tensor("w_o", [1024, 1024], F32, kind="ExternalInput").ap()
    w_rt = nc.dram_tensor("w_rt", [1024, 36], F32, kind="ExternalInput").ap()
    b_rt = nc.dram_tensor("b_rt", [128, 36], F32, kind="ExternalInput").ap()
    w_ei = nc.dram_tensor("w_ei", [32, 1024, 1024], F32, kind="ExternalInput").ap()
    w_eo = nc.dram_tensor("w_eo", [32, 512, 1024], F32, kind="ExternalInput").ap()
    out = nc.dram_tensor("out", [ntok, 1024], F32, kind="ExternalOutput").ap()
    with contextlib.ExitStack() as st:
        def sb(name, shape, dt):
            return st.enter_context(nc.sbuf_tensor(name, shape, dt))

        def ps(name, shape, dt):
            return st.enter_context(nc.psum_tensor(name, shape, dt))

        P = Prog(nc)
        O = Ops(P)
        nw1 = sb("nw1", [128, 1024], F32)
        nw2 = sb("nw2", [128, 1024], F32)
        identb = sb("identb", [128, 128], BF16)
        identf = sb("identf", [128, 128], F32)
        ones_b = sb("ones_b", [128, 128], BF16)
        ones_f = sb("ones_f", [128, 128], F32)
        h2 = sb("h2", [128, NTL, 1024], F32)
        vT = sb("vT", [128, 8, ntok], BF16)
        G = sb("G", [128, NTL, 32], F32)
        wi = sb("wi", [128, 8, 1024], BF16)
        woe = sb("woe", [128, 4, 1024], BF16)
        actT = sb("actT", [128, 4 * ntok if ntok >= 2048 else 8192], BF16)
        wgc = [[sb("wgc%d_%d" % (a, i), [128, 8, 128], BF16) for i in range(2)] for a in range(2)]
        wrt = sb("wrt", [128, 8, 36], F32)
        brt = sb("brt", [128, 36], F32)
        ubf = sb("ubf", [128, 1024], BF16)
        vf = sb("vf", [128, 1024], F32)
        vTf = sb("vTf", [128, 8, 128], F32)
        ss = [sb("ss%d" % i, [128, 1], F32) for i in range(2)]
        lnv = [sb("lnv%d" % i, [128, 1], F32) for i in range(2)]
        rstd = [sb("rstd%d" % i, [128, 1], F32) for i in range(2)]
        uT = sb("uT", [128, 8, 512], BF16)
        oab = sb("oab", [128, 4, 512], BF16)
        ohb = sb("ohb", [128, 8, 512], BF16)
        mixT = sb("mixT", [128, 8, 512], BF16)
        sa = [sb("sa%d" % i, [128, 512], F32) for i in range(2)]
        m1 = sb("m1", [128, 512], BF16)
        m2 = sb("m2", [128, 512], BF16)
        silu = [sb("silu%d" % i, [128, 512], F32) for i in range(2)]
        lg = sb("lg", [128, 36], F32)
        gsm = {n: sb("g_" + n, [128, w], F32) for n, w in [("gmax", 1), ("ngmax", 1), ("maskg", 4), ("eg", 4), ("sumg", 1), ("pg", 1), ("pen", 4), ("em", 32), ("top8", 8), ("nt1", 1), ("sel", 32), ("ex", 32), ("gx", 32), ("dsum", 1), ("coef", 1)]}
        psT = ps("psT", [128, 1024], BF16)
        B = [ps("B%d" % i, [128, 512], F32) for i in range(7)]
        rot = {"g": 0, "a": 0, "b": 0}

        def nextG():
            rot["g"] = (rot["g"] + 1) % 3
            return B[rot["g"]]

        PSTK = [("psT", i) for i in range(8)]
        wo_v = actT[:, 0:8192].rearrange("p (k c) -> p k c", k=8)
        act_v = actT[:, 0:4 * ntok].rearrange("p (c t) -> p c t", c=4)
        O.DMA("sp", nw1[:], nwb[0], [], [nw1])
        O.DMA("sp", nw2[:], nwb[1], [], [nw2])
        O.DMA("sp", wrt[:], w_rt.rearrange("(k p) c -> p k c", p=128), [], [wrt])
        O.DMA("sp", brt[:], b_rt, [], [brt])
        O.DMA("pool", wi[:], w_ho.rearrange("(k p) c -> p k c", p=128), [], [wi])
        O.DMA("pool", woe[:], w_ao.rearrange("(k p) c -> p k c", p=128), [], [woe])
        O.DMA("pool", wo_v, w_o.rearrange("(k p) c -> p k c", p=128), [], [actT])
        O.MEMSET("pool", ones_f[:], 1.0, [ones_f])
        O.MEMSET("pool", ones_b[:], 1.0, [ones_b])
        P.op("pool", lambda h: h.affine_select(out=identb[:], in_=ones_b[:], pattern=[[1, 128]], compare_op=ALU.is_equal, fill=0.0, base=0, channel_multiplier=-1), [ones_b], [identb])
        P.op("pool", lambda h: h.affine_select(out=identf[:], in_=ones_f[:], pattern=[[1, 128]], compare_op=ALU.is_equal, fill=0.0, base=0, channel_multiplier=-1), [ones_f], [identf])
        wgv = wg.rearrange("(k p) c -> p k c", p=128)
        gi = 0
        for tb in range(NTB):
            ts_ = slice(tb * 512, (tb + 1) * 512)
            O.DMA("sp", oab[:], oaT.rearrange("(c p) t -> p c t", p=128)[:, :, ts_], [], [oab])
            O.DMA("sp", ohb[:], ohT.rearrange("(c p) t -> p c t", p=128)[:, :, ts_], [], [ohb])
            for t in range(4):
                T = tb * 4 + t
                xb = T % 2
                xt = h2[:, T, :]
                O.DMA("sp", xt, x2[T * 128:(T + 1) * 128, :], [], [("h2", T)])
                P.op("dve", lambda h, xt=xt, xb=xb: h.scalar_tensor_tensor(out=vf[:], in0=xt, scalar=1.0, in1=xt, op0=ALU.mult, op1=ALU.mult, accum_out=ss[xb][:]), [("h2", T)], [vf, ss[xb]])
                O.ACT(lnv[xb][:], ss[xb][:], AF.Ln, [ss[xb]], [lnv[xb]], bias=1e-6, scale=1.0 / 1024)
                O.ACT(rstd[xb][:], lnv[xb][:], AF.Exp, [lnv[xb]], [rstd[xb]], scale=-0.5)
                O.STT(ubf[:], xt, rstd[xb][:], nw1[:], ALU.mult, ALU.mult, [("h2", T), rstd[xb], nw1], [ubf])
                for c in range(8):
                    O.TR(psT[:, c * 128:(c + 1) * 128], ubf[:, c * 128:(c + 1) * 128], identb[:], [ubf, identb], PSTK)
                O.ACOPY(uT[:, :, t * 128:(t + 1) * 128], psT[:].rearrange("p (c s) -> p c s", s=128), PSTK, [uT])
            for d in range(8):
                dsl = slice(d * 128, (d + 1) * 128)
                sig = []
                for a in range(2):
                    wb = wgc[a][gi % 2]
                    O.DMA("pool", wb[:], wgv[:, :, a * 1024 + d * 128:a * 1024 + (d + 1) * 128], [], [wb])
                    p_ = nextG()
                    for k in range(8):
                        O.MM(p_[:, :], wb[:, k, :], uT[:, k, :], k == 0, k == 7, [wb, uT], [p_])
                    s_ = sa[a]
                    O.ACT(s_[:], p_[:], AF.Exp, [p_], [s_], scale=-1.0)
                    O.TS("dve", s_[:], s_[:], 1.0, None, ALU.add, None, [s_], [s_])
                    O.RECIP(s_[:], s_[:], [s_], [s_])
                    sig.append(s_)
                gi += 1
                p_ = nextG()
                for c in range(4):
                    O.MM(p_[:, :], woe[:, c, dsl], oab[:, c, :], c == 0, c == 3, [woe, oab], [p_])
                O.TT("dve", m1[:], p_[:], sig[0][:], ALU.mult, [p_, sig[0]], [m1])
                p_ = nextG()
                for c in range(8):
                    O.MM(p_[:, :], wi[:, c, dsl], ohb[:, c, :], c == 0, c == 7, [wi, ohb], [p_])
                O.TT("dve", m2[:], p_[:], sig[1][:], ALU.mult, [p_, sig[1]], [m2])
                O.TT("pool", mixT[:, d, :], m1[:], m2[:], ALU.add, [m1, m2], [mixT])
            for t in range(4):
                T = tb * 4 + t
                xb = T % 2
                for hf in range(2):
                    hs = slice(hf * 512, (hf + 1) * 512)
                    p_ = nextG()
                    for k in range(8):
                        O.MM(p_[:, :], mixT[:, k, t * 128:(t + 1) * 128], wo_v[:, k, hs], k == 0, k == 7, [mixT, actT], [p_])
                    O.TT("dve", h2[:, T, hs], p_[:], h2[:, T, hs], ALU.add, [p_, ("h2", T)], [("h2", T)])
                hT = h2[:, T, :]
                P.op("dve", lambda h, hT=hT, xb=xb: h.scalar_tensor_tensor(out=ubf[:], in0=hT, scalar=1.0, in1=hT, op0=ALU.mult, op1=ALU.mult, accum_out=ss[xb][:]), [("h2", T)], [ubf, ss[xb]])
                O.ACT(lnv[xb][:], ss[xb][:], AF.Ln, [ss[xb]], [lnv[xb]], bias=1e-6, scale=1.0 / 1024)
                O.ACT(rstd[xb][:], lnv[xb][:], AF.Exp, [lnv[xb]], [rstd[xb]], scale=-0.5)
                O.STT(vf[:], hT, rstd[xb][:], nw2[:], ALU.mult, ALU.mult, [("h2", T), rstd[xb], nw2], [vf])
                for hf in range(2):
                    pb = B[3 + hf]
                    for c in range(4):
                        cc = hf * 4 + c
                        O.MM(pb[:, c * 128:(c + 1) * 128], vf[:, cc * 128:(cc + 1) * 128], identf[:], True, True, [vf, identf], [pb])
                    O.ACOPY(vTf[:, hf * 4:(hf + 1) * 4, :], pb[:].rearrange("p (c s) -> p c s", s=128), [pb], [vTf])
                O.CP("dve", vT[:, :, T * 128:(T + 1) * 128], vTf[:], [vTf], [("vT", T)])
                p_ = nextG()
                for k in range(8):
                    O.MM(p_[:, 0:36], vTf[:, k, :], wrt[:, k, :], k == 0, k == 7, [vTf, wrt], [p_])
                g_ = gsm
                O.TT("dve", lg[:], p_[:, 0:36], brt[:], ALU.add, [p_, brt], [lg])
                P.op("dve", lambda h: h.tensor_reduce(out=g_["gmax"][:], in_=lg[:, 0:4], axis=AX.X, op=ALU.max), [lg], [g_["gmax"]])
                O.TS("dve", g_["ngmax"][:], g_["gmax"][:], -1.0, None, ALU.mult, None, [g_["gmax"]], [g_["ngmax"]])
                O.TS("dve", g_["maskg"][:], lg[:, 0:4], g_["gmax"][:, 0:1], None, ALU.is_ge, None, [lg, g_["gmax"]], [g_["maskg"]])
                O.ACT(g_["eg"][:], lg[:, 0:4], AF.Exp, [lg, g_["ngmax"]], [g_["eg"]], bias=g_["ngmax"][:, 0:1])
                P.op("dve", lambda h: h.tensor_reduce(out=g_["sumg"][:], in_=g_["eg"][:], axis=AX.X, op=ALU.add), [g_["eg"]], [g_["sumg"]])
                O.RECIP(g_["pg"][:], g_["sumg"][:], [g_["sumg"]], [g_["pg"]])
                O.TS("dve", g_["pen"][:], g_["maskg"][:], 1e30, -1e30, ALU.mult, ALU.add, [g_["maskg"]], [g_["pen"]])
                for gq in range(4):
                    O.TS("dve", g_["em"][:, gq * 8:(gq + 1) * 8], lg[:, 4 + gq * 8:4 + (gq + 1) * 8], g_["pen"][:, gq:gq + 1], None, ALU.add, None, [lg, g_["pen"]], [g_["em"]])
                P.op("dve", lambda h: h.max(out=g_["top8"][:], in_=g_["em"][:]), [g_["em"]], [g_["top8"]])
                O.TS("dve", g_["nt1"][:], g_["top8"][:, 0:1], -1.0, None, ALU.mult, None, [g_["top8"]], [g_["nt1"]])
                O.TS("dve", g_["sel"][:], g_["em"][:], g_["top8"][:, 1:2], None, ALU.is_ge, None, [g_["em"], g_["top8"]], [g_["sel"]])
                O.ACT(g_["ex"][:], g_["em"][:], AF.Exp, [g_["em"], g_["nt1"]], [g_["ex"]], bias=g_["nt1"][:, 0:1])
                P.op("dve", lambda h: h.scalar_tensor_tensor(out=g_["gx"][:], in0=g_["sel"][:], scalar=1.0, in1=g_["ex"][:], op0=ALU.mult, op1=ALU.mult, accum_out=g_["dsum"][:]), [g_["sel"], g_["ex"]], [g_["gx"], g_["dsum"]])
                O.RECIP(g_["coef"][:], g_["dsum"][:], [g_["dsum"]], [g_["coef"]])
                O.TT("dve", g_["coef"][:], g_["coef"][:], g_["pg"][:], ALU.mult, [g_["coef"], g_["pg"]], [g_["coef"]])
                O.TS("dve", G[:, T, :], g_["gx"][:], g_["coef"][:, 0:1], None, ALU.mult, None, [g_["gx"], g_["coef"]], [("G", T)])
        for e in range(nexp):
            O.DMA("pool", wi[:], w_ei[e].rearrange("(k p) c -> p k c", p=128), [], [wi])
            O.DMA("pool", woe[:], w_eo[e].rearrange("(k p) c -> p k c", p=128), [], [woe])
            for tb in range(NTB):
                ts_ = slice(tb * 512, (tb + 1) * 512)
                vk = [("vT", tb * 4 + t) for t in range(4)]
                for c in range(4):
                    rot["a"] ^= 1
                    pa, pu = (B[0], B[1]) if rot["a"] else (B[2], B[3])
                    for k in range(8):
                        O.MM(pa[:, :], wi[:, k, c * 128:(c + 1) * 128], vT[:, k, ts_], k == 0, k == 7, [wi] + vk, [pa])
                    for k in range(8):
                        O.MM(pu[:, :], wi[:, k, 512 + c * 128:512 + (c + 1) * 128], vT[:, k, ts_], k == 0, k == 7, [wi] + vk, [pu])
                    sl_ = silu[rot["a"]]
                    O.ACT(sl_[:], pa[:], AF.Silu, [pa], [sl_])
                    O.TT("dve", act_v[:, c, ts_], sl_[:], pu[:], ALU.mult, [sl_, pu], [("act", tb)] + ([actT] if e == 0 else []))
            for T in range(NTL):
                for hf in range(2):
                    hs = slice(hf * 512, (hf + 1) * 512)
                    rot["b"] = (rot["b"] + 1) % 3
                    pb = B[4 + rot["b"]]
                    for c in range(4):
                        O.MM(pb[:, :], act_v[:, c, T * 128:(T + 1) * 128], woe[:, c, hs], c == 0, c == 3, [("act", T // 4), woe], [pb])
                    O.STT(h2[:, T, hs], pb[:], G[:, T, e:e + 1], h2[:, T, hs], ALU.mult, ALU.add, [pb, ("G", T), ("h2", T)], [("h2", T)])
        O.DMA("sp", nw1[:], nwb[2], [], [nw1])
        for T in range(NTL):
            xb = T % 2
            hT = h2[:, T, :]
            P.op("dve", lambda h, hT=hT, xb=xb: h.scalar_tensor_tensor(out=ubf[:], in0=hT, scalar=1.0, in1=hT, op0=ALU.mult, op1=ALU.mult, accum_out=ss[xb][:]), [("h2", T)], [ubf, ss[xb]])
            O.ACT(lnv[xb][:], ss[xb][:], AF.Ln, [ss[xb]], [lnv[xb]], bias=1e-6, scale=1.0 / 1024)
            O.ACT(rstd[xb][:], lnv[xb][:], AF.Exp, [lnv[xb]], [rstd[xb]], scale=-0.5)
            O.STT(vf[:], hT, rstd[xb][:], nw1[:], ALU.mult, ALU.mult, [("h2", T), rstd[xb], nw1], [vf])
            O.DMA("sp", out[T * 128:(T + 1) * 128, :], vf[:], [vf], [])
        P.emit()
    return nc


def _core_cols(g):
    return np.concatenate([np.arange(g * 128, g * 128 + 128), 512 + np.arange(g * 128, g * 128 + 128),
                           1024 + np.arange(g * 128, g * 128 + 128), 1536 + np.arange(2 * g, 2 * g + 2),
                           1544 + np.arange(g * 256, g * 256 + 256), 2568 + np.arange(g * 256, g * 256 + 256),
                           3592 + np.arange(g * 256, g * 256 + 256), 4616 + np.arange(g * 256, g * 256 + 256)])


def kernel(**inputs):
    inp = {k: np.asarray(v) for k, v in inputs.items()}
    f32 = np.float32
    x = inp["x"].astype(f32, copy=False)
    w_in = inp["w_in"][0]
    cores = list(range(8))
    nwb1 = np.ascontiguousarray(np.broadcast_to(inp["norm_mix_w"][0][None, :], (128, 1024))).astype(f32)
    hnw = np.ascontiguousarray(inp["hgrn_norm_w"][0].reshape(128, 1)).astype(f32)
    maps1 = []
    for c in cores:
        b, g = c // 4, c % 4
        xs = np.zeros((NPOS, 1024), f32)
        xs[496:512] = inp["meta_tokens"]
        xs[512:] = x[b]
        ll = inp["hgrn_lb_logits"][:, g * 256:(g + 1) * 256].reshape(2, 2, 128)
        maps1.append({
            "xs": xs,
            "w1": np.ascontiguousarray(w_in[:, _core_cols(g)]).astype(f32),
            "nwb": nwb1,
            "afb": np.ascontiguousarray(np.broadcast_to(inp["attn_forget_b"][0][2 * g:2 * g + 2][None, :], (128, 2))).astype(f32),
            "lbl": np.ascontiguousarray(ll.transpose(2, 0, 1).reshape(128, 4)).astype(f32),
            "hnw": hnw,
        })
    r1 = run_bass_kernel_spmd(build1(NB), maps1, core_ids=cores).results
    nwb = np.ascontiguousarray(np.stack([np.broadcast_to(inp[k].reshape(1, 1024), (128, 1024))
                                         for k in ["norm_mix_w", "norm_ffn_w", "norm_final_w"]])).astype(f32)
    com = {
        "wg": np.ascontiguousarray(w_in[:, 5640:7688]).astype(f32),
        "nwb": nwb,
        "w_ao": np.ascontiguousarray(inp["w_attn_out"][0]).astype(f32),
        "w_ho": np.ascontiguousarray(inp["w_hgrn_out"][0]).astype(f32),
        "w_o": np.ascontiguousarray(inp["w_o"][0]).astype(f32),
        "w_rt": np.ascontiguousarray(np.concatenate([inp["w_router_group"][0], inp["w_router_expert"][0]], 1)).astype(f32),
        "b_rt": np.ascontiguousarray(np.broadcast_to(np.concatenate([inp["b_router_group"][0], inp["b_router_expert"][0]])[None, :], (128, 36))).astype(f32),
        "w_ei": np.ascontiguousarray(inp["w_expert_in"][0]).astype(f32),
        "w_eo": np.ascontiguousarray(inp["w_expert_out"][0]).astype(f32),
    }
    maps2 = []
    for c in cores:
        b, j = c // 4, c % 4
        sl = slice(2048 * j, 2048 * (j + 1))
        m = dict(com)
        m["x2"] = np.ascontiguousarray(x[b, sl])
        m["oaT"] = np.ascontiguousarray(np.concatenate([np.asarray(r1[b * 4 + g]["oa"])[:, sl] for g in range(4)], 0))
        m["ohT"] = np.ascontiguousarray(np.concatenate([np.asarray(r1[b * 4 + g]["oh"])[:, sl] for g in range(4)], 0))
        maps2.append(m)
    r2 = run_bass_kernel_spmd(build2(2048), maps2, core_ids=cores).results
    out = np.empty((2, 8192, 1024), f32)
    for c in cores:
        b, j = c // 4, c % 4
        out[b, 2048 * j:2048 * (j + 1)] = np.asarray(r2[c]["out"])
    return out
```

```python
import contextlib
import numpy as np
from concourse.bass_utils import run_bass_kernel_spmd
import concourse.bass as bass
import concourse.mybir as mybir

F32 = mybir.dt.float32
BF16 = mybir.dt.bfloat16
I32 = mybir.dt.int32
AF = mybir.ActivationFunctionType
ALU = mybir.AluOpType
AX = mybir.AxisListType

ENGS = ["pe", "act", "dve", "pool", "sp"]
NDMASEM = 6


class _Op:
    __slots__ = ("eng", "fn", "waits", "seq", "signal", "dma_slot", "dma_n", "is_dma")


class Prog:
    def __init__(self, nc, same_eng_sync=True):
        self.nc = nc
        self.same = same_eng_sync
        self.ops = {e: [] for e in ENGS}
        self.last_w = {}
        self.readers = {}
        self.known = {e: {} for e in ENGS}
        self.dma_rot = {e: 0 for e in ENGS}
        self.dma_last = {}
        self.dma_cnt = {}

    @staticmethod
    def _key(op):
        return ("dma", op.eng, op.dma_slot) if op.is_dma else op.eng

    def _need(self, op, dep, waits):
        if dep is None:
            return
        k = self._key(dep)
        if (not dep.is_dma) and dep.eng == op.eng and (op.eng == "pe" or not self.same):
            return
        v = dep.dma_n if dep.is_dma else dep.seq
        if self.known[op.eng].get(k, 0) >= v:
            return
        cur = waits.get(k)
        if cur is None or (cur.dma_n if cur.is_dma else cur.seq) < v:
            waits[k] = dep

    @staticmethod
    def rk(r):
        if isinstance(r, (str, int)):
            return r
        if isinstance(r, tuple):
            return tuple(Prog.rk(x) for x in r)
        return id(r)

    def op(self, eng, fn, reads=(), writes=(), dma=False):
        reads = [self.rk(r) for r in reads]
        writes = [self.rk(r) for r in writes]
        o = _Op()
        o.eng = eng
        o.fn = fn
        o.is_dma = dma
        o.signal = False
        o.seq = len(self.ops[eng]) + 1
        o.dma_slot = None
        o.dma_n = 0
        waits = {}
        if dma:
            s = self.dma_rot[eng]
            self.dma_rot[eng] = (s + 1) % NDMASEM
            o.dma_slot = s
            n = self.dma_cnt.get((eng, s), 0) + 1
            self.dma_cnt[(eng, s)] = n
            o.dma_n = n
            prev = self.dma_last.get((eng, s))
            self._need(o, prev, waits)
            self.dma_last[(eng, s)] = o
        for r in reads:
            self._need(o, self.last_w.get(r), waits)
        for r in writes:
            self._need(o, self.last_w.get(r), waits)
            for d in self.readers.get(r, {}).values():
                if d is not o:
                    self._need(o, d, waits)
        for k, d in waits.items():
            d.signal = True
            self.known[eng][k] = d.dma_n if d.is_dma else d.seq
        o.waits = list(waits.values())
        for r in reads:
            self.readers.setdefault(r, {})[self._key(o)] = o
        for r in writes:
            self.last_w[r] = o
            self.readers[r] = {}
        self.ops[eng].append(o)
        return o

    def alloc_sems(self, st):
        nc = self.nc
        self.sem = {}
        for e in ENGS:
            self.sem[e] = st.enter_context(nc.semaphore("s_" + e))
            for s_ in range(NDMASEM):
                self.sem[("dma", e, s_)] = st.enter_context(nc.semaphore("d_%s%d" % (e, s_)))

    def emit(self, final_waits_eng="sp"):
        nc = self.nc
        if not hasattr(self, "val"):
            self.val = {}
            self.start = {e: 0 for e in ENGS}
            self.sigcount = {e: 0 for e in ENGS}
            self.tail = {}
        new_ops = {e: self.ops[e][self.start[e]:] for e in ENGS}
        for e in ENGS:
            for o in new_ops[e]:
                if o.is_dma:
                    o.signal = True
        for e in ENGS:
            last = None
            for o in new_ops[e]:
                if not o.is_dma:
                    last = o
            if last is not None:
                last.signal = True
                self.tail[e] = last
        tails = list(self.tail.values()) + list(self.dma_last.values())
        for e in ENGS:
            c = self.sigcount[e]
            for o in new_ops[e]:
                if o.is_dma:
                    self.val[id(o)] = 16 * o.dma_n
                elif o.signal:
                    c += 1
                    self.val[id(o)] = c
            self.sigcount[e] = c
        import contextlib
        with contextlib.ExitStack() as st:
            if not hasattr(self, "sem"):
                self.alloc_sems(st)
                own_sems = True
            else:
                own_sems = False
            sem = self.sem
            val = self.val
            block = st.enter_context(nc.Block())
            prog = self

            def run(e, h):
                for o in new_ops[e]:
                    for d in o.waits:
                        h.wait_ge(sem[prog._key(d)], val[id(d)])
                    ins = o.fn(h)
                    if o.is_dma:
                        ins.then_inc(sem[prog._key(o)], 16)
                    elif o.signal:
                        ins.then_inc(sem[e], 1)
                if final_waits_eng == "all" or e == final_waits_eng:
                    for d in tails:
                        h.wait_ge(sem[prog._key(d)], val[id(d)])

            @block.tensor
            def _(h):
                run("pe", h)

            @block.scalar
            def _(h):
                run("act", h)

            @block.vector
            def _(h):
                run("dve", h)

            @block.gpsimd
            def _(h):
                run("pool", h)

            @block.sync
            def _(h):
                run("sp", h)
            if own_sems:
                del self.sem
        for e in ENGS:
            self.start[e] = len(self.ops[e])
        if final_waits_eng == "all":
            latest = {}
            for e in ENGS:
                if self.ops[e]:
                    nd = [o for o in self.ops[e] if not o.is_dma]
                    if nd:
                        latest[e] = nd[-1].seq
            for k, o in self.dma_last.items():
                latest[("dma", k[0], k[1])] = o.dma_n
            for e in ENGS:
                for k, v in latest.items():
                    self.known[e][k] = max(self.known[e].get(k, 0), v)


NB = 17
NT = 68
NPOS = 8704
QA, KA, VA, FA, QH, FH, IH, GH = 0, 128, 256, 384, 386, 642, 898, 1154
W1C = 1410


class Ops:
    def __init__(self, P):
        self.P = P

    def MM(self, out, lhsT, rhs, st, sp, r, w):
        self.P.op("pe", lambda h: h.matmul(out, lhsT=lhsT, rhs=rhs, start=st, stop=sp), r, w)

    def TR(self, out, in_, ident, r, w):
        self.P.op("pe", lambda h: h.transpose(out, in_, ident), r, w)

    def ACT(self, out, in_, func, r, w, bias=None, scale=None):
        kw = {}
        if bias is not None:
            kw["bias"] = bias
        if scale is not None:
            kw["scale"] = scale
        self.P.op("act", lambda h: h.activation(out=out, in_=in_, func=func, **kw), r, w)

    def ACOPY(self, out, in_, r, w):
        self.P.op("act", lambda h: h.copy(out=out, in_=in_), r, w)

    def CP(self, eng, out, in_, r, w):
        if eng == "act":
            return self.ACOPY(out, in_, r, w)
        self.P.op(eng, lambda h: h.tensor_copy(out=out, in_=in_), r, w)

    def TS(self, eng, out, in0, s1, s2, op0, op1, r, w):
        if op1 is None:
            self.P.op(eng, lambda h: h.tensor_scalar(out=out, in0=in0, scalar1=s1, scalar2=None, op0=op0), r, w)
        else:
            self.P.op(eng, lambda h: h.tensor_scalar(out=out, in0=in0, scalar1=s1, scalar2=s2, op0=op0, op1=op1), r, w)

    def TT(self, eng, out, in0, in1, op, r, w):
        self.P.op(eng, lambda h: h.tensor_tensor(out=out, in0=in0, in1=in1, op=op), r, w)

    def STT(self, out, in0, scalar, in1, op0, op1, r, w):
        self.P.op("dve", lambda h: h.scalar_tensor_tensor(out=out, in0=in0, scalar=scalar, in1=in1, op0=op0, op1=op1), r, w)

    def RECIP(self, out, in_, r, w):
        self.P.op("dve", lambda h: h.reciprocal(out=out, in_=in_), r, w)

    def MEMSET(self, eng, ap, val, w):
        self.P.op(eng, lambda h: h.memset(ap, val), [], w)

    def DMA(self, q, out, in_, r, w):
        self.P.op(q, lambda h: h.dma_start(out=out, in_=in_), r, w, dma=True)


def phase1(nc, xs, w1, nwb, afb, lbl, hnw, oa, oh, nblk, nown, stride=1):
    stage = 99
    NPOS_ = nblk * 512
    NT_ = nblk * 4
    own_blocks = sorted(nblk - 1 - stride * k_ for k_ in range(nown))
    with contextlib.ExitStack() as st:
        def sb(name, shape, dt):
            return st.enter_context(nc.sbuf_tensor(name, shape, dt))

        def ps(name, shape, dt):
            return st.enter_context(nc.psum_tensor(name, shape, dt))

        P = Prog(nc)
        O = Ops(P)
        W = sb("W", [128, 8, W1C], BF16)
        nw = sb("nw", [128, 1024], F32)
        afb_sb = sb("afb_sb", [128, 8], F32)
        lbl_sb = sb("lbl_sb", [128, 16], F32)
        hnw_sb = sb("hnw_sb", [128, 1], F32)
        identb = sb("identb", [128, 128], BF16)
        identf = sb("identf", [128, 128], F32)
        ones_b = sb("ones_b", [128, 128], BF16)
        ones_f = sb("ones_f", [128, 128], F32)
        tri_f = sb("tri_f", [128, 128], F32)
        sel0 = sb("sel0", [128, 128], F32)
        rmask = sb("rmask", [128, 512], F32)
        xt = [sb("xt%d" % i, [128, 1024], F32) for i in range(4)]
        junk = sb("junk", [128, 1024], BF16)
        ubf = [sb("ubf%d" % i, [128, 1024], BF16) for i in range(4)]
        ss4 = sb("ss4", [128, 4], F32)
        ln4 = sb("ln4", [128, 4], F32)
        rstd4 = sb("rstd4", [128, 4], F32)
        fa4 = sb("fa4", [128, 4, 2], F32)
        fe4 = sb("fe4", [128, 4, 2], F32)
        L4 = sb("L4", [128, 4, 2], F32)
        car = sb("car", [128, 4, 2], F32)
        cw4 = sb("cw4", [128, 4, 2], F32)
        ss = [sb("ss%d" % i, [128, 1], F32) for i in range(2)]
        lnv = [sb("lnv%d" % i, [128, 1], F32) for i in range(2)]
        rstd = [sb("rstd%d" % i, [128, 1], F32) for i in range(2)]
        nfk = sb("nfk", [128, NT_], F32)
        uT = [sb("uT%d" % i, [128, 8, 512], BF16) for i in range(2)]
        kT = sb("kT", [128, NPOS_], BF16)
        V = sb("V", [128, NT_, 2, 128], BF16)
        cnegK = sb("cnegK", [128, NT_, 2], F32)
        cumL = sb("cumL", [128, 2], F32)
        fat = [sb("fat%d" % i, [128, 2], F32) for i in range(2)]
        Lt = [sb("Lt%d" % i, [128, 2], F32) for i in range(2)]
        qT = [sb("qT%d" % i, [128, 512], BF16) for i in range(2)]
        vh = [sb("vh%d" % i, [128, 4, 256], BF16) for i in range(2)]
        qraw = [sb("qraw%d" % i, [128, 512], BF16) for i in range(2)]
        tmp = [[sb("tmp%d_%d" % (h, i), [128, 512], F32) for i in range(4)] for h in range(2)]
        QdT = [sb("QdT%d" % i, [128, 512], BF16) for i in range(2)]
        KdT = [sb("KdT%d" % i, [128, 512], BF16) for i in range(2)]
        KddT = [sb("KddT%d" % i, [128, 512], BF16) for i in range(2)]
        bm = [sb("bm%d" % i, [128, 4], F32) for i in range(2)]
        nbm = [sb("nbm%d" % i, [128, 4], F32) for i in range(2)]
        bl = [sb("bl%d" % i, [128, 4], F32) for i in range(2)]
        ebl = [sb("ebl%d" % i, [128, 4], F32) for i in range(2)]
        ebm = [sb("ebm%d" % i, [128, 4], F32) for i in range(2)]
        eblm = [sb("eblm%d" % i, [128, 4], F32) for i in range(2)]
        gsT = [sb("gsT%d" % i, [128, 512], BF16) for i in range(2)]
        tmpg = [sb("tmpg%d" % i, [128, 512], F32) for i in range(2)]
        S = [sb("S%d" % i, [128, 128], F32) for i in range(2)]
        Sb = [sb("Sb%d" % i, [128, 128], BF16) for i in range(2)]
        KddC = [sb("KddC%d" % i, [128, 128], BF16) for i in range(2)]
        AT = [sb("AT%d" % i, [128, 128], BF16) for i in range(2)]
        oraw = [sb("oraw%d" % i, [128, 512], F32) for i in range(2)]
        sq = [sb("sq%d" % i, [128, 512], BF16) for i in range(2)]
        ohst = [sb("ohst%d" % i, [128, 512], BF16) for i in range(2)]
        lb = sb("lb", [128, 8], F32)
        lbt = sb("lbt", [128, 8], F32)
        bias_blk = sb("bias_blk", [128, NT_, 2], F32)
        ref_sb = sb("ref_sb", [128, 2], F32)
        PT = [sb("PT%d" % i, [128, 512], BF16) for i in range(2)]
        rd = sb("rd", [128, 512], F32)
        bc_sb = sb("bc_sb", [64, 512], F32)
        oast = [sb("oast%d" % i, [64, 512], BF16) for i in range(2)]
        mbias = sb("mbias", [128, 128], F32)
        dmask = [sb("dmask%d" % i, [128, 128], F32) for i in range(2)]
        psT = ps("psT", [128, 1024], BF16)
        psI = [ps("psI%d" % i, [128, 512], F32) for i in range(2)]
        psHa = ps("psHa", [128, 512], F32)
        psHb = ps("psHb", [128, 512], F32)
        psS2 = [ps("psS2%d" % i, [128, 512], F32) for i in range(2)]
        psO = ps("psO", [128, 512], F32)
        rot = {"i": 0, "s": 0}

        def nextI():
            rot["i"] ^= 1
            return psI[rot["i"]]

        def nextS():
            rot["s"] ^= 1
            return rot["s"]

        PSTK = [("psT", i) for i in range(8)]
        O.DMA("sp", nw[:], nwb, [], [nw])
        O.DMA("sp", afb_sb[:], afb, [], [afb_sb])
        O.DMA("sp", lbl_sb[:], lbl, [], [lbl_sb])
        O.DMA("sp", hnw_sb[:], hnw, [], [hnw_sb])
        O.MEMSET("pool", ones_f[:], 1.0, [ones_f])
        O.MEMSET("pool", ones_b[:], 1.0, [ones_b])
        P.op("pool", lambda h: h.affine_select(out=identb[:], in_=ones_b[:], pattern=[[1, 128]], compare_op=ALU.is_equal, fill=0.0, base=0, channel_multiplier=-1), [ones_b], [identb])
        P.op("pool", lambda h: h.affine_select(out=identf[:], in_=ones_f[:], pattern=[[1, 128]], compare_op=ALU.is_equal, fill=0.0, base=0, channel_multiplier=-1), [ones_f], [identf])
        P.op("pool", lambda h: h.affine_select(out=tri_f[:], in_=ones_f[:], pattern=[[1, 128]], compare_op=ALU.is_ge, fill=0.0, base=0, channel_multiplier=-1), [ones_f], [tri_f])
        P.op("pool", lambda h: h.affine_select(out=sel0[:], in_=ones_f[:], pattern=[[0, 128]], compare_op=ALU.is_equal, fill=0.0, base=0, channel_multiplier=1), [ones_f], [sel0])
        O.TS("pool", mbias[:], tri_f[:], 240000.0, -240000.0, ALU.mult, ALU.add, [tri_f], [mbias])
        O.MEMSET("pool", rmask[:], 1.0, [rmask])
        O.MEMSET("pool", rmask[:].rearrange("p (c s) -> p c s", s=128)[:, :, 0:1], 0.0, [rmask])
        O.MEMSET("pool", V[:], 1.0, [V])
        lv = lbl_sb[:].rearrange("p (g r h) -> p g r h", g=4, r=2)
        O.TT("dve", lbt[:].rearrange("p (g h) -> p g h", g=4), lv[:, :, 1, :], lv[:, :, 0, :], ALU.subtract, [lbl_sb], [lbt])
        O.ACT(lbt[:], lbt[:], AF.Exp, [lbt], [lbt])
        O.TS("dve", lbt[:], lbt[:], 1.0, None, ALU.add, None, [lbt], [lbt])
        O.RECIP(lb[:], lbt[:], [lbt], [lb])

        def drive(gens):
            alive = [True] * len(gens)
            while any(alive):
                for i in range(len(gens)):
                    if alive[i]:
                        try:
                            next(gens[i])
                        except StopIteration:
                            alive[i] = False

        def step(gens):
            for gq in gens:
                try:
                    next(gq)
                except StopIteration:
                    pass

        def stageA(g, blk, gi):
            UT = uT[gi % 2]
            T0 = 4 * blk
            for t in range(4):
                O.DMA("sp", xt[t][:], xs[(T0 + t) * 128:(T0 + t + 1) * 128, :], [], [xt[t]])
            for t in range(4):
                P.op("dve", lambda h, t=t: h.scalar_tensor_tensor(out=junk[:], in0=xt[t][:], scalar=1.0, in1=xt[t][:], op0=ALU.mult, op1=ALU.mult, accum_out=ss4[:, t:t + 1]), [xt[t]], [junk, ("ss4", t)])
            ssk = [("ss4", t) for t in range(4)]
            O.ACT(ln4[:], ss4[:], AF.Ln, ssk, [ln4], bias=1e-6, scale=1.0 / 1024)
            O.ACT(rstd4[:], ln4[:], AF.Exp, [ln4], [rstd4], scale=-0.5)
            O.TS("dve", nfk[:, T0:T0 + 4], ss4[:], 1e-12, -30000.0, ALU.is_lt, ALU.mult, ssk, [("nfk", blk)])
            for t in range(4):
                O.STT(ubf[t][:], xt[t][:], rstd4[:, t:t + 1], nw[:], ALU.mult, ALU.mult, [xt[t], rstd4, nw], [ubf[t]])
            for t in range(4):
                for c in range(8):
                    O.TR(psT[:, c * 128:(c + 1) * 128], ubf[t][:, c * 128:(c + 1) * 128], identb[:], [ubf[t], identb], PSTK)
                O.CP("act" if t % 2 == 0 else "dve", UT[:, :, t * 128:(t + 1) * 128], psT[:].rearrange("p (c s) -> p c s", s=128), PSTK, [UT])

        def stageB(g, blk, gi):
            if blk == 0:
                w1v = w1[g].rearrange("(k p) c -> p k c", p=128)
                for a, b in [(0, 386), (386, 898), (898, W1C)]:
                    O.DMA("pool", W[:, :, a:b], w1v[:, :, a:b], [], [W])
                O.MEMSET("pool", cumL[:], 0.0, [cumL])
                for h in range(2):
                    O.MEMSET("pool", S[h][:], 0.0, [S[h]])
            own = blk in own_blocks
            pos0 = blk * 512
            ocol = (own_blocks.index(blk) if own else 0) * 512
            ub = gi % 2
            UT = uT[ub]
            T0 = 4 * blk

            def fm_chunk(col):
                p_ = nextI()
                for k in range(8):
                    O.MM(p_[:, :], W[:, k, col:col + 128], UT[:, k, :], k == 0, k == 7, [W, UT], [p_])
                return p_

            def fh_chain(h, p_):
                t0, t1, t2, t3 = tmp[h]
                O.ACT(t0[:], p_[:], AF.Exp, [p_], [t0], scale=-1.0)
                yield
                O.ACT(t1[:], t0[:], AF.Ln, [t0, lb], [t1], bias=1.0, scale=lb[:, 2 * g + h:2 * g + h + 1])
                O.ACT(t2[:], t0[:], AF.Ln, [t0], [t2], bias=1.0)
                yield
                O.TT("dve", t1[:], t1[:], t2[:], ALU.subtract, [t1, t2], [t1])
                yield
                O.ACT(t2[:], t1[:], AF.Exp, [t1], [t2])
                P.op("dve", lambda hh, t1=t1, t3=t3: hh.tensor_tensor_scan(out=t3[:], data0=rmask[:], data1=t1[:], initial=0.0, op0=ALU.mult, op1=ALU.add), [rmask, t1], [t3])
                yield
                O.TS("dve", t2[:], t2[:], -1.0, 1.0, ALU.mult, ALU.add, [t2], [t2])
                t3v = t3[:].rearrange("p (c s) -> p c s", s=128)
                O.CP("dve", bm[h][:].rearrange("p (c o) -> p c o", o=1), t3v[:, :, 63:64], [t3], [bm[h]])
                O.CP("dve", bl[h][:].rearrange("p (c o) -> p c o", o=1), t3v[:, :, 127:128], [t3], [bl[h]])
                O.TS("dve", nbm[h][:], bm[h][:], -1.0, None, ALU.mult, None, [bm[h]], [nbm[h]])
                O.TT("dve", eblm[h][:], bl[h][:], bm[h][:], ALU.subtract, [bl[h], bm[h]], [eblm[h]])
                yield
                O.ACT(ebl[h][:], bl[h][:], AF.Exp, [bl[h]], [ebl[h]])
                O.ACT(ebm[h][:], bm[h][:], AF.Exp, [bm[h]], [ebm[h]])
                O.ACT(eblm[h][:], eblm[h][:], AF.Exp, [eblm[h]], [eblm[h]])
                for c in range(4):
                    cs = slice(c * 128, (c + 1) * 128)
                    O.ACT(t1[:, cs], t3[:, cs], AF.Exp, [t3, bm[h]], [t1], bias=bm[h][:, c:c + 1], scale=-1.0)
                yield
                O.TT("dve", KdT[h][:], t2[:], t1[:], ALU.mult, [t2, t1], [KdT[h]])
                if own:
                    for c in range(4):
                        cs = slice(c * 128, (c + 1) * 128)
                        O.ACT(t0[:, cs], t3[:, cs], AF.Exp, [t3, nbm[h]], [t0], bias=nbm[h][:, c:c + 1])
                yield
                for c in range(4):
                    cs = slice(c * 128, (c + 1) * 128)
                    O.ACT(KddT[h][:, cs], KdT[h][:, cs], AF.Copy, [KdT[h], eblm[h]], [KddT[h]], scale=eblm[h][:, c:c + 1])
                if own:
                    O.STT(QdT[h][:], qraw[h][:], float(128 ** -0.5), t0[:], ALU.mult, ALU.mult, [qraw[h], t0], [QdT[h]])
                yield

            def proj_units():
                for h in range(2 if own else 0):
                    p_ = fm_chunk(QH + h * 128)
                    O.CP("dve", qraw[h][:], p_[:], [p_], [qraw[h]])
                    yield
                p_ = fm_chunk(KA)
                O.ACOPY(kT[:, pos0:pos0 + 512], p_[:], [p_], [("kT", blk)])
                yield
                if own:
                    p_ = fm_chunk(QA)
                    O.ACOPY(qT[ub][:], p_[:], [p_], [qT[ub]])
                    yield
                for t in range(4):
                    T = T0 + t
                    p_ = nextI()
                    for k in range(8):
                        O.MM(p_[:, 0:130], UT[:, k, t * 128:(t + 1) * 128], W[:, k, VA:VA + 130], k == 0, k == 7, [W, UT], [p_])
                    O.CP("dve", V[:, T, :, 0:64], p_[:, 0:128].rearrange("p (h d) -> p h d", d=64), [p_], [("V", T)])
                    O.TT("dve", fa4[:, t, :], p_[:, 128:130], afb_sb[:, 2 * g:2 * g + 2], ALU.add, [p_, afb_sb], [("fa4", t)])
                    yield
                fak = [("fa4", t) for t in range(4)]
                O.ACT(fe4[:], fa4[:], AF.Exp, fak, [fe4], scale=-1.0)
                O.ACT(L4[:], fe4[:], AF.Ln, [fe4], [L4], bias=1.0)
                for t in range(4):
                    p_ = nextI()
                    for k in range(8):
                        O.MM(p_[:, 0:256], UT[:, k, t * 128:(t + 1) * 128], W[:, k, IH:IH + 256], k == 0, k == 7, [W, UT], [p_])
                    O.CP("act" if t % 2 else "dve", vh[ub][:, t, :], p_[:, 0:256], [p_], [vh[ub]])
                    yield
                L4f = L4[:].rearrange("p t h -> p (t h)")
                pc = nextI()
                O.MM(pc[:, 0:8], tri_f[:], L4f, True, True, [tri_f, L4], [pc])
                O.MM(pc[:, 8:16], ones_f[:], L4f, True, True, [ones_f, L4], [pc])
                O.CP("dve", car[:, 0, :], cumL[:], [cumL], [car])
                for t in range(1, 4):
                    O.TT("dve", car[:, t, :], car[:, t - 1, :], pc[:, 8 + 2 * (t - 1):8 + 2 * t], ALU.add, [car, pc], [car])
                O.TT("dve", cumL[:], car[:, 3, :], pc[:, 14:16], ALU.add, [car, pc], [cumL])
                O.TT("dve", cw4[:].rearrange("p t h -> p (t h)"), pc[:, 0:8], car[:].rearrange("p t h -> p (t h)"), ALU.add, [pc, car], [cw4])
                for h in range(2):
                    O.TT("dve", cnegK[:, T0:T0 + 4, h], cw4[:, :, h], nfk[:, T0:T0 + 4], ALU.add, [cw4, ("nfk", blk)], [("cnegK", blk, h)])
                yield
                for h in range(2 if own else 0):
                    t0 = tmpg[h]
                    p_ = fm_chunk(GH + h * 128)
                    O.ACT(t0[:], p_[:], AF.Exp, [p_], [t0], scale=-1.0)
                    O.TS("dve", t0[:], t0[:], 1.0, None, ALU.add, None, [t0], [t0])
                    O.RECIP(t0[:], t0[:], [t0], [t0])
                    O.TS("dve", gsT[h][:], t0[:], hnw_sb[:, 0:1], None, ALU.mult, None, [t0, hnw_sb], [gsT[h]])
                    yield

            pf = [fm_chunk(FH + h * 128) for h in range(2)]
            chains = [fh_chain(h, pf[h]) for h in range(2)]
            step(chains)
            for _ in proj_units():
                step(chains)
            drive(chains)

            def hgrn_tail():
                for c in range(4):
                    cs = slice(c * 128, (c + 1) * 128)
                    for h in range(2):
                        A_ = psHa[:, h * 128:(h + 1) * 128]
                        Sg = psHa[:, 256 + h * 128:256 + (h + 1) * 128]
                        O_ = psHb[:, h * 128:(h + 1) * 128]
                        pTr = psT[:, h * 128:(h + 1) * 128]
                        Vc = vh[ub][:, c, h * 128:(h + 1) * 128]
                        O.TR(pTr, KddT[h][:, cs], identb[:], [KddT[h], identb], [("psT", h)])
                        O.ACOPY(KddC[h][:], pTr, [("psT", h)], [KddC[h]])
                        if own:
                            O.TS("dve", Sb[h][:], S[h][:], ebm[h][:, c:c + 1], None, ALU.mult, None, [S[h], ebm[h]], [Sb[h]])
                            O.MM(A_, KdT[h][:, cs], QdT[h][:, cs], True, True, [KdT[h], QdT[h]], [("psHa", "A", h)])
                            O.TT("dve", AT[h][:], A_, tri_f[:], ALU.mult, [("psHa", "A", h), tri_f], [AT[h]])
                            O.MM(O_, Vc, AT[h][:], True, False, [vh[ub], AT[h]], [("psHb", h)])
                            O.MM(O_, Sb[h][:], QdT[h][:, cs], False, True, [Sb[h], QdT[h]], [("psHb", h)])
                            O.ACOPY(oraw[h][:, cs], O_, [("psHb", h)], [oraw[h]])
                        O.MM(Sg, KddC[h][:], Vc, True, True, [KddC[h], vh[ub]], [("psHa", "S", h)])
                        O.STT(S[h][:], S[h][:], ebl[h][:, c:c + 1], Sg, ALU.mult, ALU.add, [S[h], ebl[h], ("psHa", "S", h)], [S[h]])
                        yield
                if not own:
                    return
                for h in range(2):
                    t0, t1 = tmp[h][0], tmp[h][1]
                    O.TT("dve", sq[h][:], oraw[h][:], oraw[h][:], ALU.mult, [oraw[h]], [sq[h]])
                    p_ = nextI()
                    O.MM(p_[:, :], ones_b[:], sq[h][:], True, True, [ones_b, sq[h]], [p_])
                    O.ACT(t0[:], p_[:], AF.Ln, [p_], [t0], bias=1e-6, scale=1.0 / 128)
                    O.ACT(t0[:], t0[:], AF.Exp, [t0], [t0], scale=-0.5)
                    yield
                    O.TT("dve", t1[:], oraw[h][:], t0[:], ALU.mult, [oraw[h], t0], [t1])
                    O.TT("dve", ohst[h][:], t1[:], gsT[h][:], ALU.mult, [t1, gsT[h]], [ohst[h]])
                    O.DMA("sp", oh[g * 256 + h * 128:g * 256 + (h + 1) * 128, ocol:ocol + 512], ohst[h][:], [ohst[h]], ["ohS"])
                    yield

            def attention():
                ntile = 4 * blk + 4
                pr = nextI()
                O.MM(pr[:, 0:2], sel0[:], cnegK[:, 4 * blk + 2, :], True, True, [sel0, ("cnegK", blk, 0), ("cnegK", blk, 1)], [pr])
                O.ACOPY(ref_sb[:], pr[:, 0:2], [pr], [ref_sb])
                for h in range(2):
                    ck = [("cnegK", bb, h) for bb in range(0, blk + 1)]
                    O.TS("dve", bias_blk[:, 3:ntile, h], cnegK[:, 3:ntile, h], ref_sb[:, h:h + 1], None, ALU.subtract, None, ck + [ref_sb], [("bias", h)])
                yield
                for h in range(2):
                    hp = slice(h * 64, (h + 1) * 64)
                    po = psO
                    kts = list(range(3, ntile))

                    def emitS(i):
                        kt = kts[i]
                        r = kt - 4 * blk
                        c0 = 128 * r if r > 0 else 0
                        sr = i % 2
                        O.MM(psS2[sr][:, c0:512], kT[hp, kt * 128:(kt + 1) * 128], qT[ub][hp, c0:512], True, True, [("kT", kt // 4), qT[ub]], [psS2[sr]])
                        if r < 0:
                            O.ACT(PT[sr][:, c0:512], psS2[sr][:, c0:512], AF.Exp, [psS2[sr], ("bias", h)], [PT[sr]], bias=bias_blk[:, kt, h:h + 1], scale=0.125)
                        else:
                            O.TT("dve", dmask[sr][:], psS2[sr][:, c0:c0 + 128], mbias[:], ALU.add, [psS2[sr], mbias], [dmask[sr]])
                            O.ACT(PT[sr][:, c0:c0 + 128], dmask[sr][:], AF.Exp, [dmask[sr], ("bias", h)], [PT[sr]], bias=bias_blk[:, kt, h:h + 1], scale=0.125)
                            if c0 + 128 < 512:
                                O.ACT(PT[sr][:, c0 + 128:512], psS2[sr][:, c0 + 128:512], AF.Exp, [psS2[sr], ("bias", h)], [PT[sr]], bias=bias_blk[:, kt, h:h + 1], scale=0.125)

                    def emitPV(i):
                        kt = kts[i]
                        r = kt - 4 * blk
                        c0 = 128 * r if r > 0 else 0
                        sr = i % 2
                        O.MM(po[:, c0:512], V[:, kt, h, :], PT[sr][:, c0:512], i == 0, i == len(kts) - 1, [("V", kt), PT[sr]], [po])

                    emitS(0)
                    for i in range(len(kts)):
                        if i + 1 < len(kts):
                            emitS(i + 1)
                        emitPV(i)
                        yield
                    O.RECIP(rd[64:128, :], po[64:128, :], [po], [rd])
                    pn = nextI()
                    O.MM(pn[0:64, :], identf[64:128, 64:128], rd[64:128, :], True, True, [identf, rd], [pn])
                    O.ACOPY(bc_sb[:], pn[0:64, :], [pn], [bc_sb])
                    O.TT("dve", oast[h][:], po[0:64, :], bc_sb[:], ALU.mult, [po, bc_sb], [oast[h]])
                    O.DMA("sp", oa[g * 128 + h * 64:g * 128 + (h + 1) * 64, ocol:ocol + 512], oast[h][:], [oast[h]], ["oaS"])
                    yield

            if not own:
                drive([hgrn_tail()])
            else:
                ht = hgrn_tail()
                n_att = 2 * (4 * blk + 1) + 3
                every = max(1, n_att // 13)
                cnt = 0
                ht_alive = True
                for _ in attention():
                    cnt += 1
                    if ht_alive and cnt % every == 0:
                        try:
                            next(ht)
                        except StopIteration:
                            ht_alive = False
                if ht_alive:
                    drive([ht])

        order = [(g_, b_) for g_ in range(4) for b_ in range(nblk)]
        stageA(order[0][0], order[0][1], 0)
        for i, (g, blk) in enumerate(order):
            if i + 1 < len(order):
                stageA(order[i + 1][0], order[i + 1][1], i + 1)
            stageB(g, blk, i)
        P.emit(final_waits_eng="all")


def phase2(nc, x2rows, oaT, ohT, wg, nwb, w_ao, w_ho, w_o, w_rt, b_rt, w_ei, w_eo, out, ntok, nexp=32):
    NTL = ntok // 128
    NTB = ntok // 512
    with contextlib.ExitStack() as st:
        def sb(name, shape, dt):
            return st.enter_context(nc.sbuf_tensor("z_" + name, shape, dt))

        def ps(name, shape, dt):
            return st.enter_context(nc.psum_tensor("z_" + name, shape, dt))

        P = Prog(nc)
        O = Ops(P)
        nw1 = sb("nw1", [128, 1024], F32)
        nw2 = sb("nw2", [128, 1024], F32)
        identb = sb("identb", [128, 128], BF16)
        identf = sb("identf", [128, 128], F32)
        ones_b = sb("ones_b", [128, 128], BF16)
        ones_f = sb("ones_f", [128, 128], F32)
        h2 = sb("h2", [128, NTL, 1024], F32)
        vT = sb("vT", [128, 8, ntok], BF16)
        G = sb("G", [128, NTL, 32], F32)
        wi = sb("wi", [128, 8, 1024], BF16)
        woe = sb("woe", [128, 4, 1024], BF16)
        actT = sb("actT", [128, 4 * ntok if ntok >= 2048 else 8192], BF16)
        wgc = [[sb("wgc%d_%d" % (a, i), [128, 8, 128], BF16) for i in range(2)] for a in range(2)]
        wrt = sb("wrt", [128, 8, 36], F32)
        brt = sb("brt", [128, 36], F32)
        ubf = sb("ubf", [128, 1024], BF16)
        vf = sb("vf", [128, 1024], F32)
        vTf = sb("vTf", [128, 8, 128], F32)
        ss = [sb("ss%d" % i, [128, 1], F32) for i in range(2)]
        lnv = [sb("lnv%d" % i, [128, 1], F32) for i in range(2)]
        rstd = [sb("rstd%d" % i, [128, 1], F32) for i in range(2)]
        uT = sb("uT", [128, 8, 512], BF16)
        oab = sb("oab", [128, 4, 512], BF16)
        ohb = sb("ohb", [128, 8, 512], BF16)
        mixT = sb("mixT", [128, 8, 512], BF16)
        sa = [sb("sa%d" % i, [128, 512], F32) for i in range(2)]
        m1 = sb("m1", [128, 512], BF16)
        m2 = sb("m2", [128, 512], BF16)
        silu = [sb("silu%d" % i, [128, 512], F32) for i in range(2)]
        lg = sb("lg", [128, 36], F32)
        gsm = {n: sb("g_" + n, [128, w], F32) for n, w in [("gmax", 1), ("ngmax", 1), ("maskg", 4), ("eg", 4), ("sumg", 1), ("pg", 1), ("pen", 4), ("em", 32), ("top8", 8), ("nt1", 1), ("sel", 32), ("ex", 32), ("gx", 32), ("dsum", 1), ("coef", 1)]}
        psT = ps("psT", [128, 1024], BF16)
        B = [ps("B%d" % i, [128, 512], F32) for i in range(7)]
        rot = {"g": 0, "a": 0, "b": 0}

        def nextG():
            rot["g"] = (rot["g"] + 1) % 3
            return B[rot["g"]]

        PSTK = [("psT", i) for i in range(8)]
        wo_v = actT[:, 0:8192].rearrange("p (k c) -> p k c", k=8)
        act_v = actT[:, 0:4 * ntok].rearrange("p (c t) -> p c t", c=4)
        O.DMA("sp", nw1[:], nwb[0], [], [nw1])
        O.DMA("sp", nw2[:], nwb[1], [], [nw2])
        O.DMA("sp", wrt[:], w_rt.rearrange("(k p) c -> p k c", p=128), [], [wrt])
        O.DMA("sp", brt[:], b_rt, [], [brt])
        O.DMA("pool", wi[:], w_ho.rearrange("(k p) c -> p k c", p=128), [], [wi])
        O.DMA("pool", woe[:], w_ao.rearrange("(k p) c -> p k c", p=128), [], [woe])
        O.DMA("pool", wo_v, w_o.rearrange("(k p) c -> p k c", p=128), [], [actT])
        O.MEMSET("pool", ones_f[:], 1.0, [ones_f])
        O.MEMSET("pool", ones_b[:], 1.0, [ones_b])
        P.op("pool", lambda h: h.affine_select(out=identb[:], in_=ones_b[:], pattern=[[1, 128]], compare_op=ALU.is_equal, fill=0.0, base=0, channel_multiplier=-1), [ones_b], [identb])
        P.op("pool", lambda h: h.affine_select(out=identf[:], in_=ones_f[:], pattern=[[1, 128]], compare_op=ALU.is_equal, fill=0.0, base=0, channel_multiplier=-1), [ones_f], [identf])
        wgv = wg.rearrange("(k p) c -> p k c", p=128)
        gi = 0
        for tb in range(NTB):
            ts_ = slice(tb * 512, (tb + 1) * 512)
            O.DMA("sp", oab[:], oaT.rearrange("(c p) t -> p c t", p=128)[:, :, ts_], [], [oab])
            O.DMA("sp", ohb[:], ohT.rearrange("(c p) t -> p c t", p=128)[:, :, ts_], [], [ohb])
            for t in range(4):
                T = tb * 4 + t
                xb = T % 2
                xt = h2[:, T, :]
                O.DMA("sp", xt, x2rows(T), [], [("h2", T)])
                P.op("dve", lambda h, xt=xt, xb=xb: h.scalar_tensor_tensor(out=vf[:], in0=xt, scalar=1.0, in1=xt, op0=ALU.mult, op1=ALU.mult, accum_out=ss[xb][:]), [("h2", T)], [vf, ss[xb]])
                O.ACT(lnv[xb][:], ss[xb][:], AF.Ln, [ss[xb]], [lnv[xb]], bias=1e-6, scale=1.0 / 1024)
                O.ACT(rstd[xb][:], lnv[xb][:], AF.Exp, [lnv[xb]], [rstd[xb]], scale=-0.5)
                O.STT(ubf[:], xt, rstd[xb][:], nw1[:], ALU.mult, ALU.mult, [("h2", T), rstd[xb], nw1], [ubf])
                for c in range(8):
                    O.TR(psT[:, c * 128:(c + 1) * 128], ubf[:, c * 128:(c + 1) * 128], identb[:], [ubf, identb], PSTK)
                O.ACOPY(uT[:, :, t * 128:(t + 1) * 128], psT[:].rearrange("p (c s) -> p c s", s=128), PSTK, [uT])
            for d in range(8):
                dsl = slice(d * 128, (d + 1) * 128)
                sig = []
                for a in range(2):
                    wb = wgc[a][gi % 2]
                    O.DMA("pool", wb[:], wgv[:, :, a * 1024 + d * 128:a * 1024 + (d + 1) * 128], [], [wb])
                    p_ = nextG()
                    for k in range(8):
                        O.MM(p_[:, :], wb[:, k, :], uT[:, k, :], k == 0, k == 7, [wb, uT], [p_])
                    s_ = sa[a]
                    O.ACT(s_[:], p_[:], AF.Exp, [p_], [s_], scale=-1.0)
                    O.TS("dve", s_[:], s_[:], 1.0, None, ALU.add, None, [s_], [s_])
                    O.RECIP(s_[:], s_[:], [s_], [s_])
                    sig.append(s_)
                gi += 1
                p_ = nextG()
                for c in range(4):
                    O.MM(p_[:, :], woe[:, c, dsl], oab[:, c, :], c == 0, c == 3, [woe, oab], [p_])
                O.TT("dve", m1[:], p_[:], sig[0][:], ALU.mult, [p_, sig[0]], [m1])
                p_ = nextG()
                for c in range(8):
                    O.MM(p_[:, :], wi[:, c, dsl], ohb[:, c, :], c == 0, c == 7, [wi, ohb], [p_])
                O.TT("dve", m2[:], p_[:], sig[1][:], ALU.mult, [p_, sig[1]], [m2])
                O.TT("pool", mixT[:, d, :], m1[:], m2[:], ALU.add, [m1, m2], [mixT])
            for t in range(4):
                T = tb * 4 + t
                xb = T % 2
                for hf in range(2):
                    hs = slice(hf * 512, (hf + 1) * 512)
                    p_ = nextG()
                    for k in range(8):
                        O.MM(p_[:, :], mixT[:, k, t * 128:(t + 1) * 128], wo_v[:, k, hs], k == 0, k == 7, [mixT, actT], [p_])
                    O.TT("dve", h2[:, T, hs], p_[:], h2[:, T, hs], ALU.add, [p_, ("h2", T)], [("h2", T)])
                hT = h2[:, T, :]
                P.op("dve", lambda h, hT=hT, xb=xb: h.scalar_tensor_tensor(out=ubf[:], in0=hT, scalar=1.0, in1=hT, op0=ALU.mult, op1=ALU.mult, accum_out=ss[xb][:]), [("h2", T)], [ubf, ss[xb]])
                O.ACT(lnv[xb][:], ss[xb][:], AF.Ln, [ss[xb]], [lnv[xb]], bias=1e-6, scale=1.0 / 1024)
                O.ACT(rstd[xb][:], lnv[xb][:], AF.Exp, [lnv[xb]], [rstd[xb]], scale=-0.5)
                O.STT(vf[:], hT, rstd[xb][:], nw2[:], ALU.mult, ALU.mult, [("h2", T), rstd[xb], nw2], [vf])
                for hf in range(2):
                    pb = B[3 + hf]
                    for c in range(4):
                        cc = hf * 4 + c
                        O.MM(pb[:, c * 128:(c + 1) * 128], vf[:, cc * 128:(cc + 1) * 128], identf[:], True, True, [vf, identf], [pb])
                    O.ACOPY(vTf[:, hf * 4:(hf + 1) * 4, :], pb[:].rearrange("p (c s) -> p c s", s=128), [pb], [vTf])
                O.CP("dve", vT[:, :, T * 128:(T + 1) * 128], vTf[:], [vTf], [("vT", T)])
                p_ = nextG()
                for k in range(8):
                    O.MM(p_[:, 0:36], vTf[:, k, :], wrt[:, k, :], k == 0, k == 7, [vTf, wrt], [p_])
                g_ = gsm
                O.TT("dve", lg[:], p_[:, 0:36], brt[:], ALU.add, [p_, brt], [lg])
                P.op("dve", lambda h: h.tensor_reduce(out=g_["gmax"][:], in_=lg[:, 0:4], axis=AX.X, op=ALU.max), [lg], [g_["gmax"]])
                O.TS("dve", g_["ngmax"][:], g_["gmax"][:], -1.0, None, ALU.mult, None, [g_["gmax"]], [g_["ngmax"]])
                O.TS("dve", g_["maskg"][:], lg[:, 0:4], g_["gmax"][:, 0:1], None, ALU.is_ge, None, [lg, g_["gmax"]], [g_["maskg"]])
                O.ACT(g_["eg"][:], lg[:, 0:4], AF.Exp, [lg, g_["ngmax"]], [g_["eg"]], bias=g_["ngmax"][:, 0:1])
                P.op("dve", lambda h: h.tensor_reduce(out=g_["sumg"][:], in_=g_["eg"][:], axis=AX.X, op=ALU.add), [g_["eg"]], [g_["sumg"]])
                O.RECIP(g_["pg"][:], g_["sumg"][:], [g_["sumg"]], [g_["pg"]])
                O.TS("dve", g_["pen"][:], g_["maskg"][:], 1e30, -1e30, ALU.mult, ALU.add, [g_["maskg"]], [g_["pen"]])
                for gq in range(4):
                    O.TS("dve", g_["em"][:, gq * 8:(gq + 1) * 8], lg[:, 4 + gq * 8:4 + (gq + 1) * 8], g_["pen"][:, gq:gq + 1], None, ALU.add, None, [lg, g_["pen"]], [g_["em"]])
                P.op("dve", lambda h: h.max(out=g_["top8"][:], in_=g_["em"][:]), [g_["em"]], [g_["top8"]])
                O.TS("dve", g_["nt1"][:], g_["top8"][:, 0:1], -1.0, None, ALU.mult, None, [g_["top8"]], [g_["nt1"]])
                O.TS("dve", g_["sel"][:], g_["em"][:], g_["top8"][:, 1:2], None, ALU.is_ge, None, [g_["em"], g_["top8"]], [g_["sel"]])
                O.ACT(g_["ex"][:], g_["em"][:], AF.Exp, [g_["em"], g_["nt1"]], [g_["ex"]], bias=g_["nt1"][:, 0:1])
                P.op("dve", lambda h: h.scalar_tensor_tensor(out=g_["gx"][:], in0=g_["sel"][:], scalar=1.0, in1=g_["ex"][:], op0=ALU.mult, op1=ALU.mult, accum_out=g_["dsum"][:]), [g_["sel"], g_["ex"]], [g_["gx"], g_["dsum"]])
                O.RECIP(g_["coef"][:], g_["dsum"][:], [g_["dsum"]], [g_["coef"]])
                O.TT("dve", g_["coef"][:], g_["coef"][:], g_["pg"][:], ALU.mult, [g_["coef"], g_["pg"]], [g_["coef"]])
                O.TS("dve", G[:, T, :], g_["gx"][:], g_["coef"][:, 0:1], None, ALU.mult, None, [g_["gx"], g_["coef"]], [("G", T)])
        for e in range(nexp):
            O.DMA("pool", wi[:], w_ei[e].rearrange("(k p) c -> p k c", p=128), [], [wi])
            O.DMA("pool", woe[:], w_eo[e].rearrange("(k p) c -> p k c", p=128), [], [woe])
            for tb in range(NTB):
                ts_ = slice(tb * 512, (tb + 1) * 512)
                vk = [("vT", tb * 4 + t) for t in range(4)]
                for c in range(4):
                    rot["a"] ^= 1
                    pa, pu = (B[0], B[1]) if rot["a"] else (B[2], B[3])
                    for k in range(8):
                        O.MM(pa[:, :], wi[:, k, c * 128:(c + 1) * 128], vT[:, k, ts_], k == 0, k == 7, [wi] + vk, [pa])
                    for k in range(8):
                        O.MM(pu[:, :], wi[:, k, 512 + c * 128:512 + (c + 1) * 128], vT[:, k, ts_], k == 0, k == 7, [wi] + vk, [pu])
                    sl_ = silu[rot["a"]]
                    O.ACT(sl_[:], pa[:], AF.Silu, [pa], [sl_])
                    O.TT("dve", act_v[:, c, ts_], sl_[:], pu[:], ALU.mult, [sl_, pu], [("act", tb)] + ([actT] if e == 0 else []))
            for T in range(NTL):
                for hf in range(2):
                    hs = slice(hf * 512, (hf + 1) * 512)
                    rot["b"] = (rot["b"] + 1) % 3
                    pb = B[4 + rot["b"]]
                    for c in range(4):
                        O.MM(pb[:, :], act_v[:, c, T * 128:(T + 1) * 128], woe[:, c, hs], c == 0, c == 3, [("act", T // 4), woe], [pb])
                    O.STT(h2[:, T, hs], pb[:], G[:, T, e:e + 1], h2[:, T, hs], ALU.mult, ALU.add, [pb, ("G", T), ("h2", T)], [("h2", T)])
        O.DMA("sp", nw1[:], nwb[2], [], [nw1])
        for T in range(NTL):
            xb = T % 2
            hT = h2[:, T, :]
            P.op("dve", lambda h, hT=hT, xb=xb: h.scalar_tensor_tensor(out=ubf[:], in0=hT, scalar=1.0, in1=hT, op0=ALU.mult, op1=ALU.mult, accum_out=ss[xb][:]), [("h2", T)], [ubf, ss[xb]])
            O.ACT(lnv[xb][:], ss[xb][:], AF.Ln, [ss[xb]], [lnv[xb]], bias=1e-6, scale=1.0 / 1024)
            O.ACT(rstd[xb][:], lnv[xb][:], AF.Exp, [lnv[xb]], [rstd[xb]], scale=-0.5)
            O.STT(vf[:], hT, rstd[xb][:], nw1[:], ALU.mult, ALU.mult, [("h2", T), rstd[xb], nw1], [vf])
            O.DMA("sp", out[T * 128:(T + 1) * 128, :], vf[:], [vf], [])
        P.emit()


def buildF(nblk=17, nown=4, nexp=32, stride=1):
    ntok = nown * 512
    nc = bass.Bass("TRN2", target_bir_lowering=False)
    xs = nc.dram_tensor("xs", [nblk * 512, 1024], F32, kind="ExternalInput").ap()
    w1 = nc.dram_tensor("w1", [4, 1024, W1C], F32, kind="ExternalInput").ap()
    nwb = nc.dram_tensor("nwb", [3, 128, 1024], F32, kind="ExternalInput").ap()
    afb = nc.dram_tensor("afb", [128, 8], F32, kind="ExternalInput").ap()
    lbl = nc.dram_tensor("lbl", [128, 16], F32, kind="ExternalInput").ap()
    hnw = nc.dram_tensor("hnw", [128, 1], F32, kind="ExternalInput").ap()
    wg = nc.dram_tensor("wg", [1024, 2048], F32, kind="ExternalInput").ap()
    w_ao = nc.dram_tensor("w_ao", [512, 1024], F32, kind="ExternalInput").ap()
    w_ho = nc.dram_tensor("w_ho", [1024, 1024], F32, kind="ExternalInput").ap()
    w_o = nc.dram_tensor("w_o", [1024, 1024], F32, kind="ExternalInput").ap()
    w_rt = nc.dram_tensor("w_rt", [1024, 36], F32, kind="ExternalInput").ap()
    b_rt = nc.dram_tensor("b_rt", [128, 36], F32, kind="ExternalInput").ap()
    w_ei = nc.dram_tensor("w_ei", [32, 1024, 1024], F32, kind="ExternalInput").ap()
    w_eo = nc.dram_tensor("w_eo", [32, 512, 1024], F32, kind="ExternalInput").ap()
    out = nc.dram_tensor("out", [ntok, 1024], F32, kind="ExternalOutput").ap()
    oaS = nc.dram_tensor("oaS", [512, ntok], BF16, kind="Internal").ap()
    ohS = nc.dram_tensor("ohS", [1024, ntok], BF16, kind="Internal").ap()
    phase1(nc, xs, w1, nwb[0], afb, lbl, hnw, oaS, ohS, nblk, nown, stride)
    own_blocks = sorted(nblk - 1 - stride * k_ for k_ in range(nown))

    def x2rows(T):
        r0 = own_blocks[T // 4] * 512 + (T % 4) * 128
        return xs[r0:r0 + 128, :]

    phase2(nc, x2rows, oaS, ohS, wg, nwb, w_ao, w_ho, w_o, w_rt, b_rt, w_ei, w_eo, out, ntok, nexp)
    return nc


def _core_cols(g):
    return np.concatenate([np.arange(g * 128, g * 128 + 128), 512 + np.arange(g * 128, g * 128 + 128),
                           1024 + np.arange(g * 128, g * 128 + 128), 1536 + np.arange(2 * g, 2 * g + 2),
                           1544 + np.arange(g * 256, g * 256 + 256), 2568 + np.arange(g * 256, g * 256 + 256),
                           3592 + np.arange(g * 256, g * 256 + 256), 4616 + np.arange(g * 256, g * 256 + 256)])


def kernel(**inputs):
    inp = {k: np.asarray(v) for k, v in inputs.items()}
    f32 = np.float32
    x = inp["x"].astype(f32, copy=False)
    w_in = inp["w_in"][0]
    cores = list(range(8))
    ll = inp["hgrn_lb_logits"].reshape(2, 4, 2, 128)
    com = {
        "w1": np.ascontiguousarray(np.stack([w_in[:, _core_cols(g)] for g in range(4)])).astype(f32),
        "nwb": np.ascontiguousarray(np.stack([np.broadcast_to(inp[k].reshape(1, 1024), (128, 1024))
                                              for k in ["norm_mix_w", "norm_ffn_w", "norm_final_w"]])).astype(f32),
        "afb": np.ascontiguousarray(np.broadcast_to(inp["attn_forget_b"][0][None, :], (128, 8))).astype(f32),
        "lbl": np.ascontiguousarray(ll.transpose(3, 1, 0, 2).reshape(128, 16)).astype(f32),
        "hnw": np.ascontiguousarray(inp["hgrn_norm_w"][0].reshape(128, 1)).astype(f32),
        "wg": np.ascontiguousarray(w_in[:, 5640:7688]).astype(f32),
        "w_ao": np.ascontiguousarray(inp["w_attn_out"][0]).astype(f32),
        "w_ho": np.ascontiguousarray(inp["w_hgrn_out"][0]).astype(f32),
        "w_o": np.ascontiguousarray(inp["w_o"][0]).astype(f32),
        "w_rt": np.ascontiguousarray(np.concatenate([inp["w_router_group"][0], inp["w_router_expert"][0]], 1)).astype(f32),
        "b_rt": np.ascontiguousarray(np.broadcast_to(np.concatenate([inp["b_router_group"][0], inp["b_router_expert"][0]])[None, :], (128, 36))).astype(f32),
        "w_ei": np.ascontiguousarray(inp["w_expert_in"][0]).astype(f32),
        "w_eo": np.ascontiguousarray(inp["w_expert_out"][0]).astype(f32),
    }
    maps = []
    for c in cores:
        b, j = c // 4, c % 4
        R = 512 * (13 + j)
        xs = np.zeros((NPOS, 1024), f32)
        xs[NPOS - R - 16:NPOS - R] = inp["meta_tokens"]
        xs[NPOS - R:] = x[b, :R]
        m = dict(com)
        m["xs"] = xs
        maps.append(m)
    res = run_bass_kernel_spmd(buildF(NB, 4, 32, 4), maps, core_ids=cores).results
    out = np.empty((2, 8192, 1024), f32)
    for c in cores:
        b, j = c // 4, c % 4
        o = np.asarray(res[c]["out"])
        for ob in range(4):
            out[b, (4 * ob + j) * 512:(4 * ob + j + 1) * 512] = o[ob * 512:(ob + 1) * 512]
    return out
```

```python
import contextlib
import numpy as np
from concourse.bass_utils import run_bass_kernel_spmd
import concourse.bass as bass
import concourse.mybir as mybir

F32 = mybir.dt.float32
BF16 = mybir.dt.bfloat16
I32 = mybir.dt.int32
AF = mybir.ActivationFunctionType
ALU = mybir.AluOpType
AX = mybir.AxisListType

ENGS = ["pe", "act", "dve", "pool", "sp"]
NDMASEM = 6


class _Op:
    __slots__ = ("eng", "fn", "waits", "seq", "signal", "dma_slot", "dma_n", "is_dma")


class Prog:
    def __init__(self, nc, same_eng_sync=True):
        self.nc = nc
        self.same = same_eng_sync
        self.ops = {e: [] for e in ENGS}
        self.last_w = {}
        self.readers = {}
        self.known = {e: {} for e in ENGS}
        self.dma_rot = {e: 0 for e in ENGS}
        self.dma_last = {}
        self.dma_cnt = {}

    @staticmethod
    def _key(op):
        return ("dma", op.eng, op.dma_slot) if op.is_dma else op.eng

    def _need(self, op, dep, waits):
        if dep is None:
            return
        k = self._key(dep)
        if (not dep.is_dma) and dep.eng == op.eng and (op.eng == "pe" or not self.same):
            return
        v = dep.dma_n if dep.is_dma else dep.seq
        if self.known[op.eng].get(k, 0) >= v:
            return
        cur = waits.get(k)
        if cur is None or (cur.dma_n if cur.is_dma else cur.seq) < v:
            waits[k] = dep

    @staticmethod
    def rk(r):
        if isinstance(r, (str, int)):
            return r
        if isinstance(r, tuple):
            return tuple(Prog.rk(x) for x in r)
        return id(r)

    def op(self, eng, fn, reads=(), writes=(), dma=False):
        reads = [self.rk(r) for r in reads]
        writes = [self.rk(r) for r in writes]
        o = _Op()
        o.eng = eng
        o.fn = fn
        o.is_dma = dma
        o.signal = False
        o.seq = len(self.ops[eng]) + 1
        o.dma_slot = None
        o.dma_n = 0
        waits = {}
        if dma:
            s = self.dma_rot[eng]
            self.dma_rot[eng] = (s + 1) % NDMASEM
            o.dma_slot = s
            n = self.dma_cnt.get((eng, s), 0) + 1
            self.dma_cnt[(eng, s)] = n
            o.dma_n = n
            prev = self.dma_last.get((eng, s))
            self._need(o, prev, waits)
            self.dma_last[(eng, s)] = o
        for r in reads:
            self._need(o, self.last_w.get(r), waits)
        for r in writes:
            self._need(o, self.last_w.get(r), waits)
            for d in self.readers.get(r, {}).values():
                if d is not o:
                    self._need(o, d, waits)
        for k, d in waits.items():
            d.signal = True
            self.known[eng][k] = d.dma_n if d.is_dma else d.seq
        o.waits = list(waits.values())
        for r in reads:
            self.readers.setdefault(r, {})[self._key(o)] = o
        for r in writes:
            self.last_w[r] = o
            self.readers[r] = {}
        self.ops[eng].append(o)
        return o

    def alloc_sems(self, st):
        nc = self.nc
        self.sem = {}
        for e in ENGS:
            self.sem[e] = st.enter_context(nc.semaphore("s_" + e))
            for s_ in range(NDMASEM):
                self.sem[("dma", e, s_)] = st.enter_context(nc.semaphore("d_%s%d" % (e, s_)))

    def emit(self, final_waits_eng="sp"):
        nc = self.nc
        if not hasattr(self, "val"):
            self.val = {}
            self.start = {e: 0 for e in ENGS}
            self.sigcount = {e: 0 for e in ENGS}
            self.tail = {}
        new_ops = {e: self.ops[e][self.start[e]:] for e in ENGS}
        for e in ENGS:
            for o in new_ops[e]:
                if o.is_dma:
                    o.signal = True
        for e in ENGS:
            last = None
            for o in new_ops[e]:
                if not o.is_dma:
                    last = o
            if last is not None:
                last.signal = True
                self.tail[e] = last
        tails = list(self.tail.values()) + list(self.dma_last.values())
        for e in ENGS:
            c = self.sigcount[e]
            for o in new_ops[e]:
                if o.is_dma:
                    self.val[id(o)] = 16 * o.dma_n
                elif o.signal:
                    c += 1
                    self.val[id(o)] = c
            self.sigcount[e] = c
        import contextlib
        with contextlib.ExitStack() as st:
            if not hasattr(self, "sem"):
                self.alloc_sems(st)
                own_sems = True
            else:
                own_sems = False
            sem = self.sem
            val = self.val
            block = st.enter_context(nc.Block())
            prog = self

            def run(e, h):
                for o in new_ops[e]:
                    for d in o.waits:
                        h.wait_ge(sem[prog._key(d)], val[id(d)])
                    ins = o.fn(h)
                    if o.is_dma:
                        ins.then_inc(sem[prog._key(o)], 16)
                    elif o.signal:
                        ins.then_inc(sem[e], 1)
                if final_waits_eng == "all" or e == final_waits_eng:
                    for d in tails:
                        h.wait_ge(sem[prog._key(d)], val[id(d)])

            @block.tensor
            def _(h):
                run("pe", h)

            @block.scalar
            def _(h):
                run("act", h)

            @block.vector
            def _(h):
                run("dve", h)

            @block.gpsimd
            def _(h):
                run("pool", h)

            @block.sync
            def _(h):
                run("sp", h)
            if own_sems:
                del self.sem
        for e in ENGS:
            self.start[e] = len(self.ops[e])
        if final_waits_eng == "all":
            latest = {}
            for e in ENGS:
                if self.ops[e]:
                    nd = [o for o in self.ops[e] if not o.is_dma]
                    if nd:
                        latest[e] = nd[-1].seq
            for k, o in self.dma_last.items():
                latest[("dma", k[0], k[1])] = o.dma_n
            for e in ENGS:
                for k, v in latest.items():
                    self.known[e][k] = max(self.known[e].get(k, 0), v)


NB = 17
NT = 68
NPOS = 8704
QA, KA, VA, FA, QH, FH, IH, GH = 0, 128, 256, 384, 386, 642, 898, 1154
W1C = 1410


class Ops:
    def __init__(self, P):
        self.P = P

    def MM(self, out, lhsT, rhs, st, sp, r, w):
        self.P.op("pe", lambda h: h.matmul(out, lhsT=lhsT, rhs=rhs, start=st, stop=sp), r, w)

    def TR(self, out, in_, ident, r, w):
        self.P.op("pe", lambda h: h.transpose(out, in_, ident), r, w)

    def ACT(self, out, in_, func, r, w, bias=None, scale=None):
        kw = {}
        if bias is not None:
            kw["bias"] = bias
        if scale is not None:
            kw["scale"] = scale
        self.P.op("act", lambda h: h.activation(out=out, in_=in_, func=func, **kw), r, w)

    def ACOPY(self, out, in_, r, w):
        self.P.op("act", lambda h: h.copy(out=out, in_=in_), r, w)

    def CP(self, eng, out, in_, r, w):
        if eng == "act":
            return self.ACOPY(out, in_, r, w)
        self.P.op(eng, lambda h: h.tensor_copy(out=out, in_=in_), r, w)

    def TS(self, eng, out, in0, s1, s2, op0, op1, r, w):
        if op1 is None:
            self.P.op(eng, lambda h: h.tensor_scalar(out=out, in0=in0, scalar1=s1, scalar2=None, op0=op0), r, w)
        else:
            self.P.op(eng, lambda h: h.tensor_scalar(out=out, in0=in0, scalar1=s1, scalar2=s2, op0=op0, op1=op1), r, w)

    def TT(self, eng, out, in0, in1, op, r, w):
        self.P.op(eng, lambda h: h.tensor_tensor(out=out, in0=in0, in1=in1, op=op), r, w)

    def STT(self, out, in0, scalar, in1, op0, op1, r, w):
        self.P.op("dve", lambda h: h.scalar_tensor_tensor(out=out, in0=in0, scalar=scalar, in1=in1, op0=op0, op1=op1), r, w)

    def RECIP(self, out, in_, r, w):
        self.P.op("dve", lambda h: h.reciprocal(out=out, in_=in_), r, w)

    def MEMSET(self, eng, ap, val, w):
        self.P.op(eng, lambda h: h.memset(ap, val), [], w)

    def DMA(self, q, out, in_, r, w):
        self.P.op(q, lambda h: h.dma_start(out=out, in_=in_), r, w, dma=True)


def phase1(nc, xs, w1, nwb, afb, lbl, hnw, oa, oh, nblk, nown, stride=1):
    stage = 99
    NPOS_ = nblk * 512
    NT_ = nblk * 4
    own_blocks = sorted(nblk - 1 - stride * k_ for k_ in range(nown))
    with contextlib.ExitStack() as st:
        def sb(name, shape, dt):
            return st.enter_context(nc.sbuf_tensor(name, shape, dt))

        def ps(name, shape, dt):
            return st.enter_context(nc.psum_tensor(name, shape, dt))

        P = Prog(nc)
        O = Ops(P)
        W = sb("W", [128, 8, W1C], BF16)
        nw = sb("nw", [128, 1024], F32)
        afb_sb = sb("afb_sb", [128, 8], F32)
        lbl_sb = sb("lbl_sb", [128, 16], F32)
        hnw_sb = sb("hnw_sb", [128, 1], F32)
        identb = sb("identb", [128, 128], BF16)
        identf = sb("identf", [128, 128], F32)
        ones_b = sb("ones_b", [128, 128], BF16)
        ones_f = sb("ones_f", [128, 128], F32)
        tri_f = sb("tri_f", [128, 128], F32)
        sel0 = sb("sel0", [128, 128], F32)
        rmask = sb("rmask", [128, 512], F32)
        xt = [sb("xt%d" % i, [128, 1024], F32) for i in range(4)]
        junk = sb("junk", [128, 1024], BF16)
        ubf = [sb("ubf%d" % i, [128, 1024], BF16) for i in range(4)]
        ss4 = sb("ss4", [128, 4], F32)
        ln4 = sb("ln4", [128, 4], F32)
        rstd4 = sb("rstd4", [128, 4], F32)
        fa4 = sb("fa4", [128, 4, 2], F32)
        fe4 = sb("fe4", [128, 4, 2], F32)
        L4 = sb("L4", [128, 4, 2], F32)
        car = sb("car", [128, 4, 2], F32)
        cw4 = sb("cw4", [128, 4, 2], F32)
        ss = [sb("ss%d" % i, [128, 1], F32) for i in range(2)]
        lnv = [sb("lnv%d" % i, [128, 1], F32) for i in range(2)]
        rstd = [sb("rstd%d" % i, [128, 1], F32) for i in range(2)]
        nfk = sb("nfk", [128, NT_], F32)
        uT = [sb("uT%d" % i, [128, 8, 512], BF16) for i in range(2)]
        kT = sb("kT", [128, NPOS_], BF16)
        V = sb("V", [128, NT_, 2, 128], BF16)
        cnegK = sb("cnegK", [128, NT_, 2], F32)
        cumL = sb("cumL", [128, 2], F32)
        fat = [sb("fat%d" % i, [128, 2], F32) for i in range(2)]
        Lt = [sb("Lt%d" % i, [128, 2], F32) for i in range(2)]
        qT = [sb("qT%d" % i, [128, 512], BF16) for i in range(2)]
        vh = [sb("vh%d" % i, [128, 4, 256], BF16) for i in range(2)]
        qraw = [sb("qraw%d" % i, [128, 512], BF16) for i in range(2)]
        tmp = [[sb("tmp%d_%d" % (h, i), [128, 512], F32) for i in range(4)] for h in range(2)]
        QdT = [sb("QdT%d" % i, [128, 512], BF16) for i in range(2)]
        KdT = [sb("KdT%d" % i, [128, 512], BF16) for i in range(2)]
        KddT = [sb("KddT%d" % i, [128, 512], BF16) for i in range(2)]
        bm = [sb("bm%d" % i, [128, 4], F32) for i in range(2)]
        nbm = [sb("nbm%d" % i, [128, 4], F32) for i in range(2)]
        bl = [sb("bl%d" % i, [128, 4], F32) for i in range(2)]
        ebl = [sb("ebl%d" % i, [128, 4], F32) for i in range(2)]
        ebm = [sb("ebm%d" % i, [128, 4], F32) for i in range(2)]
        eblm = [sb("eblm%d" % i, [128, 4], F32) for i in range(2)]
        gsT = [sb("gsT%d" % i, [128, 512], BF16) for i in range(2)]
        tmpg = [sb("tmpg%d" % i, [128, 512], F32) for i in range(2)]
        S = [sb("S%d" % i, [128, 128], F32) for i in range(2)]
        Sb = [sb("Sb%d" % i, [128, 128], BF16) for i in range(2)]
        KddC = [sb("KddC%d" % i, [128, 128], BF16) for i in range(2)]
        AT = [sb("AT%d" % i, [128, 128], BF16) for i in range(2)]
        oraw = [sb("oraw%d" % i, [128, 512], F32) for i in range(2)]
        sq = [sb("sq%d" % i, [128, 512], BF16) for i in range(2)]
        ohst = [sb("ohst%d" % i, [128, 512], BF16) for i in range(2)]
        lb = sb("lb", [128, 8], F32)
        lbt = sb("lbt", [128, 8], F32)
        bias_blk = sb("bias_blk", [128, NT_, 2], F32)
        ref_sb = sb("ref_sb", [128, 2], F32)
        PT = [sb("PT%d" % i, [128, 512], BF16) for i in range(2)]
        rd = sb("rd", [128, 512], F32)
        bc_sb = sb("bc_sb", [64, 512], F32)
        oast = [sb("oast%d" % i, [64, 512], BF16) for i in range(2)]
        psT = ps("psT", [128, 1024], BF16)
        psI = [ps("psI%d" % i, [128, 512], F32) for i in range(2)]
        psHa = ps("psHa", [128, 512], F32)
        psHb = ps("psHb", [128, 512], F32)
        psS2 = [ps("psS2%d" % i, [128, 512], F32) for i in range(2)]
        psO = ps("psO", [128, 512], F32)
        rot = {"i": 0, "s": 0}

        def nextI():
            rot["i"] ^= 1
            return psI[rot["i"]]

        def nextS():
            rot["s"] ^= 1
            return rot["s"]

        PSTK = [("psT", i) for i in range(8)]
        O.DMA("sp", nw[:], nwb, [], [nw])
        O.DMA("sp", afb_sb[:], afb, [], [afb_sb])
        O.DMA("sp", lbl_sb[:], lbl, [], [lbl_sb])
        O.DMA("sp", hnw_sb[:], hnw, [], [hnw_sb])
        O.MEMSET("pool", ones_f[:], 1.0, [ones_f])
        O.MEMSET("pool", ones_b[:], 1.0, [ones_b])
        P.op("pool", lambda h: h.affine_select(out=identb[:], in_=ones_b[:], pattern=[[1, 128]], compare_op=ALU.is_equal, fill=0.0, base=0, channel_multiplier=-1), [ones_b], [identb])
        P.op("pool", lambda h: h.affine_select(out=identf[:], in_=ones_f[:], pattern=[[1, 128]], compare_op=ALU.is_equal, fill=0.0, base=0, channel_multiplier=-1), [ones_f], [identf])
        P.op("pool", lambda h: h.affine_select(out=tri_f[:], in_=ones_f[:], pattern=[[1, 128]], compare_op=ALU.is_ge, fill=0.0, base=0, channel_multiplier=-1), [ones_f], [tri_f])
        P.op("pool", lambda h: h.affine_select(out=sel0[:], in_=ones_f[:], pattern=[[0, 128]], compare_op=ALU.is_equal, fill=0.0, base=0, channel_multiplier=1), [ones_f], [sel0])
        O.MEMSET("pool", rmask[:], 1.0, [rmask])
        O.MEMSET("pool", rmask[:].rearrange("p (c s) -> p c s", s=128)[:, :, 0:1], 0.0, [rmask])
        O.MEMSET("pool", V[:], 1.0, [V])
        lv = lbl_sb[:].rearrange("p (g r h) -> p g r h", g=4, r=2)
        O.TT("dve", lbt[:].rearrange("p (g h) -> p g h", g=4), lv[:, :, 1, :], lv[:, :, 0, :], ALU.subtract, [lbl_sb], [lbt])
        O.ACT(lbt[:], lbt[:], AF.Exp, [lbt], [lbt])
        O.TS("dve", lbt[:], lbt[:], 1.0, None, ALU.add, None, [lbt], [lbt])
        O.RECIP(lb[:], lbt[:], [lbt], [lb])

        def drive(gens):
            alive = [True] * len(gens)
            while any(alive):
                for i in range(len(gens)):
                    if alive[i]:
                        try:
                            next(gens[i])
                        except StopIteration:
                            alive[i] = False

        def step(gens):
            for gq in gens:
                try:
                    next(gq)
                except StopIteration:
                    pass

        def stageA(g, blk, gi):
            UT = uT[gi % 2]
            T0 = 4 * blk
            for t in range(4):
                O.DMA("sp", xt[t][:], xs[(T0 + t) * 128:(T0 + t + 1) * 128, :], [], [xt[t]])
            for t in range(4):
                P.op("dve", lambda h, t=t: h.scalar_tensor_tensor(out=junk[:], in0=xt[t][:], scalar=1.0, in1=xt[t][:], op0=ALU.mult, op1=ALU.mult, accum_out=ss4[:, t:t + 1]), [xt[t]], [junk, ("ss4", t)])
            ssk = [("ss4", t) for t in range(4)]
            O.ACT(ln4[:], ss4[:], AF.Ln, ssk, [ln4], bias=1e-6, scale=1.0 / 1024)
            O.ACT(rstd4[:], ln4[:], AF.Exp, [ln4], [rstd4], scale=-0.5)
            O.TS("dve", nfk[:, T0:T0 + 4], ss4[:], 1e-12, -30000.0, ALU.is_lt, ALU.mult, ssk, [("nfk", blk)])
            for t in range(4):
                O.STT(ubf[t][:], xt[t][:], rstd4[:, t:t + 1], nw[:], ALU.mult, ALU.mult, [xt[t], rstd4, nw], [ubf[t]])
            for t in range(4):
                for c in range(8):
                    O.TR(psT[:, c * 128:(c + 1) * 128], ubf[t][:, c * 128:(c + 1) * 128], identb[:], [ubf[t], identb], PSTK)
                O.CP("act" if t % 2 == 0 else "dve", UT[:, :, t * 128:(t + 1) * 128], psT[:].rearrange("p (c s) -> p c s", s=128), PSTK, [UT])

        def stageB(g, blk, gi):
            if blk == 0:
                w1v = w1[g].rearrange("(k p) c -> p k c", p=128)
                for a, b in [(0, 386), (386, 898), (898, W1C)]:
                    O.DMA("pool", W[:, :, a:b], w1v[:, :, a:b], [], [W])
                O.MEMSET("pool", cumL[:], 0.0, [cumL])
                for h in range(2):
                    O.MEMSET("pool", S[h][:], 0.0, [S[h]])
            own = blk in own_blocks
            pos0 = blk * 512
            ocol = (own_blocks.index(blk) if own else 0) * 512
            ub = gi % 2
            UT = uT[ub]
            T0 = 4 * blk

            def fm_chunk(col):
                p_ = nextI()
                for k in range(8):
                    O.MM(p_[:, :], W[:, k, col:col + 128], UT[:, k, :], k == 0, k == 7, [W, UT], [p_])
                return p_

            def fh_chain(h, p_):
                t0, t1, t2, t3 = tmp[h]
                O.ACT(t0[:], p_[:], AF.Exp, [p_], [t0], scale=-1.0)
                yield
                O.ACT(t1[:], t0[:], AF.Ln, [t0, lb], [t1], bias=1.0, scale=lb[:, 2 * g + h:2 * g + h + 1])
                O.ACT(t2[:], t0[:], AF.Ln, [t0], [t2], bias=1.0)
                yield
                O.TT("dve", t1[:], t1[:], t2[:], ALU.subtract, [t1, t2], [t1])
                yield
                O.ACT(t2[:], t1[:], AF.Exp, [t1], [t2])
                P.op("dve", lambda hh, t1=t1, t3=t3: hh.tensor_tensor_scan(out=t3[:], data0=rmask[:], data1=t1[:], initial=0.0, op0=ALU.mult, op1=ALU.add), [rmask, t1], [t3])
                yield
                O.TS("dve", t2[:], t2[:], -1.0, 1.0, ALU.mult, ALU.add, [t2], [t2])
                t3v = t3[:].rearrange("p (c s) -> p c s", s=128)
                O.CP("dve", bm[h][:].rearrange("p (c o) -> p c o", o=1), t3v[:, :, 63:64], [t3], [bm[h]])
                O.CP("dve", bl[h][:].rearrange("p (c o) -> p c o", o=1), t3v[:, :, 127:128], [t3], [bl[h]])
                O.TS("dve", nbm[h][:], bm[h][:], -1.0, None, ALU.mult, None, [bm[h]], [nbm[h]])
                O.TT("dve", eblm[h][:], bl[h][:], bm[h][:], ALU.subtract, [bl[h], bm[h]], [eblm[h]])
                yield
                O.ACT(ebl[h][:], bl[h][:], AF.Exp, [bl[h]], [ebl[h]])
                O.ACT(ebm[h][:], bm[h][:], AF.Exp, [bm[h]], [ebm[h]])
                O.ACT(eblm[h][:], eblm[h][:], AF.Exp, [eblm[h]], [eblm[h]])
                for c in range(4):
                    cs = slice(c * 128, (c + 1) * 128)
                    O.ACT(t1[:, cs], t3[:, cs], AF.Exp, [t3, bm[h]], [t1], bias=bm[h][:, c:c + 1], scale=-1.0)
                yield
                O.TT("dve", KdT[h][:], t2[:], t1[:], ALU.mult, [t2, t1], [KdT[h]])
                if own:
                    for c in range(4):
                        cs = slice(c * 128, (c + 1) * 128)
                        O.ACT(t0[:, cs], t3[:, cs], AF.Exp, [t3, nbm[h]], [t0], bias=nbm[h][:, c:c + 1])
                yield
                for c in range(4):
                    cs = slice(c * 128, (c + 1) * 128)
                    O.ACT(KddT[h][:, cs], KdT[h][:, cs], AF.Copy, [KdT[h], eblm[h]], [KddT[h]], scale=eblm[h][:, c:c + 1])
                if own:
                    O.STT(QdT[h][:], qraw[h][:], float(128 ** -0.5), t0[:], ALU.mult, ALU.mult, [qraw[h], t0], [QdT[h]])
                yield

            def proj_units():
                for h in range(2 if own else 0):
                    p_ = fm_chunk(QH + h * 128)
                    O.CP("dve", qraw[h][:], p_[:], [p_], [qraw[h]])
                    yield
                p_ = fm_chunk(KA)
                O.ACOPY(kT[:, pos0:pos0 + 512], p_[:], [p_], [("kT", blk)])
                yield
                if own:
                    p_ = fm_chunk(QA)
                    O.ACOPY(qT[ub][:], p_[:], [p_], [qT[ub]])
                    yield
                for t in range(4):
                    T = T0 + t
                    p_ = nextI()
                    for k in range(8):
                        O.MM(p_[:, 0:130], UT[:, k, t * 128:(t + 1) * 128], W[:, k, VA:VA + 130], k == 0, k == 7, [W, UT], [p_])
                    O.CP("dve", V[:, T, :, 0:64], p_[:, 0:128].rearrange("p (h d) -> p h d", d=64), [p_], [("V", T)])
                    O.TT("dve", fa4[:, t, :], p_[:, 128:130], afb_sb[:, 2 * g:2 * g + 2], ALU.add, [p_, afb_sb], [("fa4", t)])
                    yield
                fak = [("fa4", t) for t in range(4)]
                O.ACT(fe4[:], fa4[:], AF.Exp, fak, [fe4], scale=-1.0)
                O.ACT(L4[:], fe4[:], AF.Ln, [fe4], [L4], bias=1.0)
                for t in range(4):
                    p_ = nextI()
                    for k in range(8):
                        O.MM(p_[:, 0:256], UT[:, k, t * 128:(t + 1) * 128], W[:, k, IH:IH + 256], k == 0, k == 7, [W, UT], [p_])
                    O.CP("act" if t % 2 else "dve", vh[ub][:, t, :], p_[:, 0:256], [p_], [vh[ub]])
                    yield
                L4f = L4[:].rearrange("p t h -> p (t h)")
                pc = nextI()
                O.MM(pc[:, 0:8], tri_f[:], L4f, True, True, [tri_f, L4], [pc])
                O.MM(pc[:, 8:16], ones_f[:], L4f, True, True, [ones_f, L4], [pc])
                O.CP("dve", car[:, 0, :], cumL[:], [cumL], [car])
                for t in range(1, 4):
                    O.TT("dve", car[:, t, :], car[:, t - 1, :], pc[:, 8 + 2 * (t - 1):8 + 2 * t], ALU.add, [car, pc], [car])
                O.TT("dve", cumL[:], car[:, 3, :], pc[:, 14:16], ALU.add, [car, pc], [cumL])
                O.TT("dve", cw4[:].rearrange("p t h -> p (t h)"), pc[:, 0:8], car[:].rearrange("p t h -> p (t h)"), ALU.add, [pc, car], [cw4])
                for h in range(2):
                    O.TT("dve", cnegK[:, T0:T0 + 4, h], cw4[:, :, h], nfk[:, T0:T0 + 4], ALU.add, [cw4, ("nfk", blk)], [("cnegK", blk, h)])
                yield
                for h in range(2 if own else 0):
                    t0 = tmpg[h]
                    p_ = fm_chunk(GH + h * 128)
                    O.ACT(t0[:], p_[:], AF.Exp, [p_], [t0], scale=-1.0)
                    O.TS("dve", t0[:], t0[:], 1.0, None, ALU.add, None, [t0], [t0])
                    O.RECIP(t0[:], t0[:], [t0], [t0])
                    O.TS("dve", gsT[h][:], t0[:], hnw_sb[:, 0:1], None, ALU.mult, None, [t0, hnw_sb], [gsT[h]])
                    yield

            pf = [fm_chunk(FH + h * 128) for h in range(2)]
            chains = [fh_chain(h, pf[h]) for h in range(2)]
            step(chains)
            for _ in proj_units():
                step(chains)
            drive(chains)

            def hgrn_tail():
                for c in range(4):
                    cs = slice(c * 128, (c + 1) * 128)
                    for h in range(2):
                        A_ = psHa[:, h * 128:(h + 1) * 128]
                        Sg = psHa[:, 256 + h * 128:256 + (h + 1) * 128]
                        O_ = psHb[:, h * 128:(h + 1) * 128]
                        pTr = psT[:, h * 128:(h + 1) * 128]
                        Vc = vh[ub][:, c, h * 128:(h + 1) * 128]
                        O.TR(pTr, KddT[h][:, cs], identb[:], [KddT[h], identb], [("psT", h)])
                        O.ACOPY(KddC[h][:], pTr, [("psT", h)], [KddC[h]])
                        if own:
                            O.TS("dve", Sb[h][:], S[h][:], ebm[h][:, c:c + 1], None, ALU.mult, None, [S[h], ebm[h]], [Sb[h]])
                            O.MM(A_, KdT[h][:, cs], QdT[h][:, cs], True, True, [KdT[h], QdT[h]], [("psHa", "A", h)])
                            O.TT("dve", AT[h][:], A_, tri_f[:], ALU.mult, [("psHa", "A", h), tri_f], [AT[h]])
                            O.MM(O_, Vc, AT[h][:], True, False, [vh[ub], AT[h]], [("psHb", h)])
                            O.MM(O_, Sb[h][:], QdT[h][:, cs], False, True, [Sb[h], QdT[h]], [("psHb", h)])
                            O.ACOPY(oraw[h][:, cs], O_, [("psHb", h)], [oraw[h]])
                        O.MM(Sg, KddC[h][:], Vc, True, True, [KddC[h], vh[ub]], [("psHa", "S", h)])
                        O.STT(S[h][:], S[h][:], ebl[h][:, c:c + 1], Sg, ALU.mult, ALU.add, [S[h], ebl[h], ("psHa", "S", h)], [S[h]])
                        yield
                if not own:
                    return
                for h in range(2):
                    t0, t1 = tmp[h][0], tmp[h][1]
                    O.TT("dve", sq[h][:], oraw[h][:], oraw[h][:], ALU.mult, [oraw[h]], [sq[h]])
                    p_ = nextI()
                    O.MM(p_[:, :], ones_b[:], sq[h][:], True, True, [ones_b, sq[h]], [p_])
                    O.ACT(t0[:], p_[:], AF.Ln, [p_], [t0], bias=1e-6, scale=1.0 / 128)
                    O.ACT(t0[:], t0[:], AF.Exp, [t0], [t0], scale=-0.5)
                    yield
                    O.TT("dve", t1[:], oraw[h][:], t0[:], ALU.mult, [oraw[h], t0], [t1])
                    O.TT("dve", ohst[h][:], t1[:], gsT[h][:], ALU.mult, [t1, gsT[h]], [ohst[h]])
                    O.DMA("sp", oh[g * 256 + h * 128:g * 256 + (h + 1) * 128, ocol:ocol + 512], ohst[h][:], [ohst[h]], ["ohS"])
                    yield

            def attention():
                ntile = 4 * blk + 4
                pr = nextI()
                O.MM(pr[:, 0:2], sel0[:], cnegK[:, 4 * blk + 2, :], True, True, [sel0, ("cnegK", blk, 0), ("cnegK", blk, 1)], [pr])
                O.ACOPY(ref_sb[:], pr[:, 0:2], [pr], [ref_sb])
                for h in range(2):
                    ck = [("cnegK", bb, h) for bb in range(0, blk + 1)]
                    O.TS("dve", bias_blk[:, 3:ntile, h], cnegK[:, 3:ntile, h], ref_sb[:, h:h + 1], None, ALU.subtract, None, ck + [ref_sb], [("bias", h)])
                yield
                for h in range(2):
                    hp = slice(h * 64, (h + 1) * 64)
                    po = psO
                    kts = list(range(3, ntile))

                    def emitS(i):
                        kt = kts[i]
                        r = kt - 4 * blk
                        c0 = 128 * r if r > 0 else 0
                        sr = i % 2
                        O.MM(psS2[sr][:, c0:512], kT[hp, kt * 128:(kt + 1) * 128], qT[ub][hp, c0:512], True, True, [("kT", kt // 4), qT[ub]], [psS2[sr]])
                        O.ACT(PT[sr][:, c0:512], psS2[sr][:, c0:512], AF.Exp, [psS2[sr], ("bias", h)], [PT[sr]], bias=bias_blk[:, kt, h:h + 1], scale=0.125)
                        if r >= 0:
                            O.TT("pool", PT[sr][:, c0:c0 + 128], PT[sr][:, c0:c0 + 128], tri_f[:], ALU.mult, [PT[sr], tri_f], [PT[sr]])

                    def emitPV(i):
                        kt = kts[i]
                        r = kt - 4 * blk
                        c0 = 128 * r if r > 0 else 0
                        sr = i % 2
                        O.MM(po[:, c0:512], V[:, kt, h, :], PT[sr][:, c0:512], i == 0, i == len(kts) - 1, [("V", kt), PT[sr]], [po])

                    emitS(0)
                    for i in range(len(kts)):
                        if i + 1 < len(kts):
                            emitS(i + 1)
                        emitPV(i)
                        yield
                    O.RECIP(rd[64:128, :], po[64:128, :], [po], [rd])
                    pn = nextI()
                    O.MM(pn[0:64, :], identf[64:128, 64:128], rd[64:128, :], True, True, [identf, rd], [pn])
                    O.ACOPY(bc_sb[:], pn[0:64, :], [pn], [bc_sb])
                    O.TT("dve", oast[h][:], po[0:64, :], bc_sb[:], ALU.mult, [po, bc_sb], [oast[h]])
                    O.DMA("sp", oa[g * 128 + h * 64:g * 128 + (h + 1) * 64, ocol:ocol + 512], oast[h][:], [oast[h]], ["oaS"])
                    yield

            if not own:
                drive([hgrn_tail()])
            else:
                ht = hgrn_tail()
                n_att = 2 * (4 * blk + 1) + 3
                every = max(1, n_att // 13)
                cnt = 0
                ht_alive = True
                for _ in attention():
                    cnt += 1
                    if ht_alive and cnt % every == 0:
                        try:
                            next(ht)
                        except StopIteration:
                            ht_alive = False
                if ht_alive:
                    drive([ht])

        order = [(g_, b_) for g_ in range(4) for b_ in range(nblk)]
        stageA(order[0][0], order[0][1], 0)
        for i, (g, blk) in enumerate(order):
            if i + 1 < len(order):
                stageA(order[i + 1][0], order[i + 1][1], i + 1)
            stageB(g, blk, i)
        P.emit(final_waits_eng="all")


def phase2(nc, x2rows, oaT, ohT, wg, nwb, w_ao, w_ho, w_o, w_rt, b_rt, w_ei, w_eo, out, ntok, nexp=32):
    NTL = ntok // 128
    NTB = ntok // 512
    with contextlib.ExitStack() as st:
        def sb(name, shape, dt):
            return st.enter_context(nc.sbuf_tensor("z_" + name, shape, dt))

        def ps(name, shape, dt):
            return st.enter_context(nc.psum_tensor("z_" + name, shape, dt))

        P = Prog(nc)
        O = Ops(P)
        nw1 = sb("nw1", [128, 1024], F32)
        nw2 = sb("nw2", [128, 1024], F32)
        identb = sb("identb", [128, 128], BF16)
        identf = sb("identf", [128, 128], F32)
        ones_b = sb("ones_b", [128, 128], BF16)
        ones_f = sb("ones_f", [128, 128], F32)
        h2 = sb("h2", [128, NTL, 1024], F32)
        vT = sb("vT", [128, 8, ntok], BF16)
        G = sb("G", [128, NTL, 32], F32)
        wi = sb("wi", [128, 8, 1024], BF16)
        woe = sb("woe", [128, 4, 1024], BF16)
        actT = sb("actT", [128, 4 * ntok if ntok >= 2048 else 8192], BF16)
        wgc = [[sb("wgc%d_%d" % (a, i), [128, 8, 128], BF16) for i in range(2)] for a in range(2)]
        wrt = sb("wrt", [128, 8, 36], F32)
        brt = sb("brt", [128, 36], F32)
        ubf = sb("ubf", [128, 1024], BF16)
        vf = sb("vf", [128, 1024], F32)
        vTf = sb("vTf", [128, 8, 128], F32)
        ss = [sb("ss%d" % i, [128, 1], F32) for i in range(2)]
        lnv = [sb("lnv%d" % i, [128, 1], F32) for i in range(2)]
        rstd = [sb("rstd%d" % i, [128, 1], F32) for i in range(2)]
        uT = sb("uT", [128, 8, 512], BF16)
        oab = sb("oab", [128, 4, 512], BF16)
        ohb = sb("ohb", [128, 8, 512], BF16)
        mixT = sb("mixT", [128, 8, 512], BF16)
        sa = [sb("sa%d" % i, [128, 512], F32) for i in range(2)]
        m1 = sb("m1", [128, 512], BF16)
        m2 = sb("m2", [128, 512], BF16)
        silu = [sb("silu%d" % i, [128, 512], F32) for i in range(1)]
        lg4 = [sb("lg%d" % i, [128, 36], F32) for i in range(4)]
        gsm4 = [{n: sb("g%d_" % i + n, [128, w], F32) for n, w in [("gmax", 1), ("ngmax", 1), ("maskg", 4), ("eg", 4), ("sumg", 1), ("pg", 1), ("pen", 4), ("em", 32), ("top8", 8), ("nt1", 1), ("sel", 32), ("ex", 32), ("gx", 32), ("dsum", 1), ("coef", 1)]} for i in range(4)]
        psT = ps("psT", [128, 1024], BF16)
        B = [ps("B%d" % i, [128, 512], F32) for i in range(7)]
        rot = {"g": 0, "a": 0, "b": 0}

        def nextG():
            rot["g"] = (rot["g"] + 1) % 3
            return B[rot["g"]]

        PSTK = [("psT", i) for i in range(8)]
        wo_v = actT[:, 0:8192].rearrange("p (k c) -> p k c", k=8)
        act_v = actT[:, 0:4 * ntok].rearrange("p (c t) -> p c t", c=4)
        O.DMA("sp", nw1[:], nwb[0], [], [nw1])
        O.DMA("sp", nw2[:], nwb[1], [], [nw2])
        O.DMA("sp", wrt[:], w_rt.rearrange("(k p) c -> p k c", p=128), [], [wrt])
        O.DMA("sp", brt[:], b_rt, [], [brt])
        O.DMA("pool", wi[:], w_ho.rearrange("(k p) c -> p k c", p=128), [], [wi])
        O.DMA("pool", woe[:], w_ao.rearrange("(k p) c -> p k c", p=128), [], [woe])
        O.DMA("pool", wo_v, w_o.rearrange("(k p) c -> p k c", p=128), [], [actT])
        O.MEMSET("pool", ones_f[:], 1.0, [ones_f])
        O.MEMSET("pool", ones_b[:], 1.0, [ones_b])
        P.op("pool", lambda h: h.affine_select(out=identb[:], in_=ones_b[:], pattern=[[1, 128]], compare_op=ALU.is_equal, fill=0.0, base=0, channel_multiplier=-1), [ones_b], [identb])
        P.op("pool", lambda h: h.affine_select(out=identf[:], in_=ones_f[:], pattern=[[1, 128]], compare_op=ALU.is_equal, fill=0.0, base=0, channel_multiplier=-1), [ones_f], [identf])
        wgv = wg.rearrange("(k p) c -> p k c", p=128)
        gi = 0
        def gate_chain(T, lg, g_):
            P.op("dve", lambda h: h.tensor_reduce(out=g_["gmax"][:], in_=lg[:, 0:4], axis=AX.X, op=ALU.max), [lg], [g_["gmax"]])
            yield
            O.TS("dve", g_["ngmax"][:], g_["gmax"][:], -1.0, None, ALU.mult, None, [g_["gmax"]], [g_["ngmax"]])
            O.TS("dve", g_["maskg"][:], lg[:, 0:4], g_["gmax"][:, 0:1], None, ALU.is_ge, None, [lg, g_["gmax"]], [g_["maskg"]])
            yield
            O.ACT(g_["eg"][:], lg[:, 0:4], AF.Exp, [lg, g_["ngmax"]], [g_["eg"]], bias=g_["ngmax"][:, 0:1])
            O.TS("dve", g_["pen"][:], g_["maskg"][:], 1e30, -1e30, ALU.mult, ALU.add, [g_["maskg"]], [g_["pen"]])
            yield
            for gq in range(4):
                O.TS("dve", g_["em"][:, gq * 8:(gq + 1) * 8], lg[:, 4 + gq * 8:4 + (gq + 1) * 8], g_["pen"][:, gq:gq + 1], None, ALU.add, None, [lg, g_["pen"]], [g_["em"]])
            yield
            P.op("dve", lambda h: h.max(out=g_["top8"][:], in_=g_["em"][:]), [g_["em"]], [g_["top8"]])
            P.op("dve", lambda h: h.tensor_reduce(out=g_["sumg"][:], in_=g_["eg"][:], axis=AX.X, op=ALU.add), [g_["eg"]], [g_["sumg"]])
            yield
            O.TS("dve", g_["nt1"][:], g_["top8"][:, 0:1], -1.0, None, ALU.mult, None, [g_["top8"]], [g_["nt1"]])
            O.TS("dve", g_["sel"][:], g_["em"][:], g_["top8"][:, 1:2], None, ALU.is_ge, None, [g_["em"], g_["top8"]], [g_["sel"]])
            O.RECIP(g_["pg"][:], g_["sumg"][:], [g_["sumg"]], [g_["pg"]])
            yield
            O.ACT(g_["ex"][:], g_["em"][:], AF.Exp, [g_["em"], g_["nt1"]], [g_["ex"]], bias=g_["nt1"][:, 0:1])
            yield
            P.op("dve", lambda h: h.scalar_tensor_tensor(out=g_["gx"][:], in0=g_["sel"][:], scalar=1.0, in1=g_["ex"][:], op0=ALU.mult, op1=ALU.mult, accum_out=g_["dsum"][:]), [g_["sel"], g_["ex"]], [g_["gx"], g_["dsum"]])
            yield
            O.RECIP(g_["coef"][:], g_["dsum"][:], [g_["dsum"]], [g_["coef"]])
            yield
            O.TT("dve", g_["coef"][:], g_["coef"][:], g_["pg"][:], ALU.mult, [g_["coef"], g_["pg"]], [g_["coef"]])
            yield
            O.TS("dve", G[:, T, :], g_["gx"][:], g_["coef"][:, 0:1], None, ALU.mult, None, [g_["gx"], g_["coef"]], [("G", T)])
            yield

        for tb in range(NTB):
            gate_gens = []
            ts_ = slice(tb * 512, (tb + 1) * 512)
            O.DMA("sp", oab[:], oaT.rearrange("(c p) t -> p c t", p=128)[:, :, ts_], [], [oab])
            O.DMA("sp", ohb[:], ohT.rearrange("(c p) t -> p c t", p=128)[:, :, ts_], [], [ohb])
            for t in range(4):
                T = tb * 4 + t
                xb = T % 2
                xt = h2[:, T, :]
                O.DMA("sp", xt, x2rows(T), [], [("h2", T)])
                P.op("dve", lambda h, xt=xt, xb=xb: h.scalar_tensor_tensor(out=vf[:], in0=xt, scalar=1.0, in1=xt, op0=ALU.mult, op1=ALU.mult, accum_out=ss[xb][:]), [("h2", T)], [vf, ss[xb]])
                O.ACT(lnv[xb][:], ss[xb][:], AF.Ln, [ss[xb]], [lnv[xb]], bias=1e-6, scale=1.0 / 1024)
                O.ACT(rstd[xb][:], lnv[xb][:], AF.Exp, [lnv[xb]], [rstd[xb]], scale=-0.5)
                O.STT(ubf[:], xt, rstd[xb][:], nw1[:], ALU.mult, ALU.mult, [("h2", T), rstd[xb], nw1], [ubf])
                for c in range(8):
                    O.TR(psT[:, c * 128:(c + 1) * 128], ubf[:, c * 128:(c + 1) * 128], identb[:], [ubf, identb], PSTK)
                O.ACOPY(uT[:, :, t * 128:(t + 1) * 128], psT[:].rearrange("p (c s) -> p c s", s=128), PSTK, [uT])
            for d in range(8):
                dsl = slice(d * 128, (d + 1) * 128)
                sig = []
                for a in range(2):
                    wb = wgc[a][gi % 2]
                    O.DMA("pool", wb[:], wgv[:, :, a * 1024 + d * 128:a * 1024 + (d + 1) * 128], [], [wb])
                    p_ = nextG()
                    for k in range(8):
                        O.MM(p_[:, :], wb[:, k, :], uT[:, k, :], k == 0, k == 7, [wb, uT], [p_])
                    s_ = sa[a]
                    O.ACT(s_[:], p_[:], AF.Exp, [p_], [s_], scale=-1.0)
                    O.TS("dve", s_[:], s_[:], 1.0, None, ALU.add, None, [s_], [s_])
                    O.RECIP(s_[:], s_[:], [s_], [s_])
                    sig.append(s_)
                gi += 1
                p_ = nextG()
                for c in range(4):
                    O.MM(p_[:, :], woe[:, c, dsl], oab[:, c, :], c == 0, c == 3, [woe, oab], [p_])
                O.TT("dve", m1[:], p_[:], sig[0][:], ALU.mult, [p_, sig[0]], [m1])
                p_ = nextG()
                for c in range(8):
                    O.MM(p_[:, :], wi[:, c, dsl], ohb[:, c, :], c == 0, c == 7, [wi, ohb], [p_])
                O.TT("dve", m2[:], p_[:], sig[1][:], ALU.mult, [p_, sig[1]], [m2])
                O.TT("pool", mixT[:, d, :], m1[:], m2[:], ALU.add, [m1, m2], [mixT])
            for t in range(4):
                T = tb * 4 + t
                xb = T % 2
                for hf in range(2):
                    hs = slice(hf * 512, (hf + 1) * 512)
                    p_ = nextG()
                    for k in range(8):
                        O.MM(p_[:, :], mixT[:, k, t * 128:(t + 1) * 128], wo_v[:, k, hs], k == 0, k == 7, [mixT, actT], [p_])
                    O.TT("dve", h2[:, T, hs], p_[:], h2[:, T, hs], ALU.add, [p_, ("h2", T)], [("h2", T)])
                hT = h2[:, T, :]
                P.op("dve", lambda h, hT=hT, xb=xb: h.scalar_tensor_tensor(out=ubf[:], in0=hT, scalar=1.0, in1=hT, op0=ALU.mult, op1=ALU.mult, accum_out=ss[xb][:]), [("h2", T)], [ubf, ss[xb]])
                O.ACT(lnv[xb][:], ss[xb][:], AF.Ln, [ss[xb]], [lnv[xb]], bias=1e-6, scale=1.0 / 1024)
                O.ACT(rstd[xb][:], lnv[xb][:], AF.Exp, [lnv[xb]], [rstd[xb]], scale=-0.5)
                O.STT(vf[:], hT, rstd[xb][:], nw2[:], ALU.mult, ALU.mult, [("h2", T), rstd[xb], nw2], [vf])
                for hf in range(2):
                    pb = B[3 + hf]
                    for c in range(4):
                        cc = hf * 4 + c
                        O.MM(pb[:, c * 128:(c + 1) * 128], vf[:, cc * 128:(cc + 1) * 128], identf[:], True, True, [vf, identf], [pb])
                    O.ACOPY(vTf[:, hf * 4:(hf + 1) * 4, :], pb[:].rearrange("p (c s) -> p c s", s=128), [pb], [vTf])
                O.CP("dve", vT[:, :, T * 128:(T + 1) * 128], vTf[:], [vTf], [("vT", T)])
                p_ = nextG()
                for k in range(8):
                    O.MM(p_[:, 0:36], vTf[:, k, :], wrt[:, k, :], k == 0, k == 7, [vTf, wrt], [p_])
                O.TT("dve", lg4[t][:], p_[:, 0:36], brt[:], ALU.add, [p_, brt], [lg4[t]])
                gate_gens.append(gate_chain(T, lg4[t], gsm4[t]))
            alive_ = [True] * len(gate_gens)
            while any(alive_):
                for gi_ in range(len(gate_gens)):
                    if alive_[gi_]:
                        try:
                            next(gate_gens[gi_])
                        except StopIteration:
                            alive_[gi_] = False
        for e in range(nexp):
            O.DMA("pool", wi[:], w_ei[e].rearrange("(k p) c -> p k c", p=128), [], [wi])
            O.DMA("pool", woe[:], w_eo[e].rearrange("(k p) c -> p k c", p=128), [], [woe])
            for tb in range(NTB):
                ts_ = slice(tb * 512, (tb + 1) * 512)
                vk = [("vT", tb * 4 + t) for t in range(4)]
                for c in range(4):
                    rot["a"] ^= 1
                    pa, pu = (B[0], B[1]) if rot["a"] else (B[2], B[3])
                    for k in range(8):
                        O.MM(pa[:, :], wi[:, k, c * 128:(c + 1) * 128], vT[:, k, ts_], k == 0, k == 7, [wi] + vk, [pa])
                    for k in range(8):
                        O.MM(pu[:, :], wi[:, k, 512 + c * 128:512 + (c + 1) * 128], vT[:, k, ts_], k == 0, k == 7, [wi] + vk, [pu])
                    sl_ = silu[0]
                    O.ACT(sl_[:], pa[:], AF.Silu, [pa], [sl_])
                    O.TT("dve", act_v[:, c, ts_], sl_[:], pu[:], ALU.mult, [sl_, pu], [("act", tb)] + ([actT] if e == 0 else []))
            for T in range(NTL):
                for hf in range(2):
                    hs = slice(hf * 512, (hf + 1) * 512)
                    rot["b"] = (rot["b"] + 1) % 3
                    pb = B[4 + rot["b"]]
                    for c in range(4):
                        O.MM(pb[:, :], act_v[:, c, T * 128:(T + 1) * 128], woe[:, c, hs], c == 0, c == 3, [("act", T // 4), woe], [pb])
                    O.STT(h2[:, T, hs], pb[:], G[:, T, e:e + 1], h2[:, T, hs], ALU.mult, ALU.add, [pb, ("G", T), ("h2", T)], [("h2", T)])
        O.DMA("sp", nw1[:], nwb[2], [], [nw1])
        for T in range(NTL):
            xb = T % 2
            hT = h2[:, T, :]
            P.op("dve", lambda h, hT=hT, xb=xb: h.scalar_tensor_tensor(out=ubf[:], in0=hT, scalar=1.0, in1=hT, op0=ALU.mult, op1=ALU.mult, accum_out=ss[xb][:]), [("h2", T)], [ubf, ss[xb]])
            O.ACT(lnv[xb][:], ss[xb][:], AF.Ln, [ss[xb]], [lnv[xb]], bias=1e-6, scale=1.0 / 1024)
            O.ACT(rstd[xb][:], lnv[xb][:], AF.Exp, [lnv[xb]], [rstd[xb]], scale=-0.5)
            O.STT(vf[:], hT, rstd[xb][:], nw1[:], ALU.mult, ALU.mult, [("h2", T), rstd[xb], nw1], [vf])
            O.DMA("sp", out[T * 128:(T + 1) * 128, :], vf[:], [vf], [])
        P.emit()


def buildF(nblk=17, nown=4, nexp=32, stride=1):
    ntok = nown * 512
    nc = bass.Bass("TRN2", target_bir_lowering=False)
    xs = nc.dram_tensor("xs", [nblk * 512, 1024], F32, kind="ExternalInput").ap()
    w1 = nc.dram_tensor("w1", [4, 1024, W1C], F32, kind="ExternalInput").ap()
    nwb = nc.dram_tensor("nwb", [3, 128, 1024], F32, kind="ExternalInput").ap()
    afb = nc.dram_tensor("afb", [128, 8], F32, kind="ExternalInput").ap()
    lbl = nc.dram_tensor("lbl", [128, 16], F32, kind="ExternalInput").ap()
    hnw = nc.dram_tensor("hnw", [128, 1], F32, kind="ExternalInput").ap()
    wg = nc.dram_tensor("wg", [1024, 2048], F32, kind="ExternalInput").ap()
    w_ao = nc.dram_tensor("w_ao", [512, 1024], F32, kind="ExternalInput").ap()
    w_ho = nc.dram_tensor("w_ho", [1024, 1024], F32, kind="ExternalInput").ap()
    w_o = nc.dram_tensor("w_o", [1024, 1024], F32, kind="ExternalInput").ap()
    w_rt = nc.dram_tensor("w_rt", [1024, 36], F32, kind="ExternalInput").ap()
    b_rt = nc.dram_tensor("b_rt", [128, 36], F32, kind="ExternalInput").ap()
    w_ei = nc.dram_tensor("w_ei", [32, 1024, 1024], F32, kind="ExternalInput").ap()
    w_eo = nc.dram_tensor("w_eo", [32, 512, 1024], F32, kind="ExternalInput").ap()
    out = nc.dram_tensor("out", [ntok, 1024], F32, kind="ExternalOutput").ap()
    oaS = nc.dram_tensor("oaS", [512, ntok], BF16, kind="Internal").ap()
    ohS = nc.dram_tensor("ohS", [1024, ntok], BF16, kind="Internal").ap()
    phase1(nc, xs, w1, nwb[0], afb, lbl, hnw, oaS, ohS, nblk, nown, stride)
    own_blocks = sorted(nblk - 1 - stride * k_ for k_ in range(nown))

    def x2rows(T):
        r0 = own_blocks[T // 4] * 512 + (T % 4) * 128
        return xs[r0:r0 + 128, :]

    phase2(nc, x2rows, oaS, ohS, wg, nwb, w_ao, w_ho, w_o, w_rt, b_rt, w_ei, w_eo, out, ntok, nexp)
    return nc


def _core_cols(g):
    return np.concatenate([np.arange(g * 128, g * 128 + 128), 512 + np.arange(g * 128, g * 128 + 128),
                           1024 + np.arange(g * 128, g * 128 + 128), 1536 + np.arange(2 * g, 2 * g + 2),
                           1544 + np.arange(g * 256, g * 256 + 256), 2568 + np.arange(g * 256, g * 256 + 256),
                           3592 + np.arange(g * 256, g * 256 + 256), 4616 + np.arange(g * 256, g * 256 + 256)])


def kernel(**inputs):
    inp = {k: np.asarray(v) for k, v in inputs.items()}
    f32 = np.float32
    x = inp["x"].astype(f32, copy=False)
    w_in = inp["w_in"][0]
    cores = list(range(8))
    ll = inp["hgrn_lb_logits"].reshape(2, 4, 2, 128)
    com = {
        "w1": np.ascontiguousarray(np.stack([w_in[:, _core_cols(g)] for g in range(4)])).astype(f32),
        "nwb": np.ascontiguousarray(np.stack([np.broadcast_to(inp[k].reshape(1, 1024), (128, 1024))
                                              for k in ["norm_mix_w", "norm_ffn_w", "norm_final_w"]])).astype(f32),
        "afb": np.ascontiguousarray(np.broadcast_to(inp["attn_forget_b"][0][None, :], (128, 8))).astype(f32),
        "lbl": np.ascontiguousarray(ll.transpose(3, 1, 0, 2).reshape(128, 16)).astype(f32),
        "hnw": np.ascontiguousarray(inp["hgrn_norm_w"][0].reshape(128, 1)).astype(f32),
        "wg": np.ascontiguousarray(w_in[:, 5640:7688]).astype(f32),
        "w_ao": np.ascontiguousarray(inp["w_attn_out"][0]).astype(f32),
        "w_ho": np.ascontiguousarray(inp["w_hgrn_out"][0]).astype(f32),
        "w_o": np.ascontiguousarray(inp["w_o"][0]).astype(f32),
        "w_rt": np.ascontiguousarray(np.concatenate([inp["w_router_group"][0], inp["w_router_expert"][0]], 1)).astype(f32),
        "b_rt": np.ascontiguousarray(np.broadcast_to(np.concatenate([inp["b_router_group"][0], inp["b_router_expert"][0]])[None, :], (128, 36))).astype(f32),
        "w_ei": np.ascontiguousarray(inp["w_expert_in"][0]).astype(f32),
        "w_eo": np.ascontiguousarray(inp["w_expert_out"][0]).astype(f32),
    }
    maps = []
    for c in cores:
        b, j = c // 4, c % 4
        R = 512 * (13 + j)
        xs = np.zeros((NPOS, 1024), f32)
        xs[NPOS - R - 16:NPOS - R] = inp["meta_tokens"]
        xs[NPOS - R:] = x[b, :R]
        m = dict(com)
        m["xs"] = xs
        maps.append(m)
    res = run_bass_kernel_spmd(buildF(NB, 4, 32, 4), maps, core_ids=cores).results
    out = np.empty((2, 8192, 1024), f32)
    for c in cores:
        b, j = c // 4, c % 4
        o = np.asarray(res[c]["out"])
        for ob in range(4):
            out[b, (4 * ob + j) * 512:(4 * ob + j + 1) * 512] = o[ob * 512:(ob + 1) * 512]
    return out
```
